# Optimizing a Trainium2 kernel written in Bass

```python
import math
import jax
import jax.numpy as jnp
from jax import lax
import numpy as np

D_MODEL = 1024
BATCH = 4
SEQ = 4096
DEPTH = 2

GRID_W = 64
CTX_LEN = 256
EPS = 1e-6

DIFF_HEADS = 6
DIFF_DH = 64
DIFF_QK = DIFF_HEADS * 2 * DIFF_DH
DIFF_V = DIFF_HEADS * 2 * DIFF_DH
FOURIER_GROUPS = 4
FOURIER_GC = 64
FOURIER_W = FOURIER_GROUPS * FOURIER_GC
EVEN_IN = 2 * DIFF_QK + DIFF_V + FOURIER_W
EVEN_MIX = DIFF_V + FOURIER_W
Q_BLOCK = 128
ROPE_BASE = 10000.0
ROPE_PAIRS = DIFF_DH // 4

CHUNK = 128
SGU_GROUPS = 8
SGU_GC = 128
SGU_W = SGU_GROUPS * SGU_GC
ODD_IN = 2 * SGU_W

PEER_HEADS = 8
N_KEYS = 128
N_EXPERTS = N_KEYS * N_KEYS
PEER_TOPK = 16
PEER_DQ = 256
PEER_HALF = PEER_DQ // 2
PEER_BLOCK = 128

N_EVEN = (DEPTH + 1) // 2
N_ODD = DEPTH // 2

kernel_name = 'hybrid_diffattn_fourier_sgu_peer_dit'


def rmsnorm(t, g):
    tf = t.astype(jnp.float32)
    tf = tf * lax.rsqrt(jnp.mean(tf * tf, axis=-1, keepdims=True) + EPS)
    return (tf * g.astype(jnp.float32)).astype(t.dtype)


def axial_rope_tables(rows, dtype):
    r = jnp.repeat(jnp.arange(rows, dtype=jnp.float32), GRID_W)
    col = jnp.tile(jnp.arange(GRID_W, dtype=jnp.float32), rows)
    inv = ROPE_BASE ** (-jnp.arange(ROPE_PAIRS, dtype=jnp.float32) / ROPE_PAIRS)
    ar = r[:, None] * inv
    ac = col[:, None] * inv
    ang = jnp.concatenate([ar, ar, ac, ac], axis=-1)
    return (jnp.cos(ang).astype(dtype)[:, None, None, :],
            jnp.sin(ang).astype(dtype)[:, None, None, :])


def rope_axial(t, cos, sin):
    a, b, c, d = jnp.split(t, 4, axis=-1)
    rot = jnp.concatenate([-b, a, -d, c], axis=-1)
    return t * cos + rot * sin


def diff_attend(q, k, v, lam):
    s = jnp.einsum('bqhmd,bkhmd->bhmqk', q, k).astype(jnp.float32) * (DIFF_DH ** -0.5)
    p = jax.nn.softmax(s, axis=-1)
    a = p[:, :, 0] - lam * p[:, :, 1]
    return jnp.einsum('bhqk,bkhe->bqhe', a.astype(v.dtype), v)


def fourier_mix(f):
    B, L, _ = f.shape
    ff = f.astype(jnp.float32).reshape(B, L, FOURIER_GROUPS, FOURIER_GC)
    y = jnp.fft.fft2(ff, axes=(1, 3), norm='ortho').real
    return y.reshape(B, L, FOURIER_W).astype(f.dtype)


def even_mixer(h, hc, w_in, w_out, lam_p, head_g, layer_num, cos, sin, update_ctx):
    B, L, _ = h.shape
    C = hc.shape[1]
    lam_init = 0.8 - 0.6 * math.exp(-0.3 * (layer_num - 1))
    lp = lam_p.astype(jnp.float32)
    lam = jnp.exp(jnp.dot(lp[0], lp[1])) - jnp.exp(jnp.dot(lp[2], lp[3])) + lam_init
    q, k, v, f = jnp.split(h @ w_in, [DIFF_QK, 2 * DIFF_QK, 2 * DIFF_QK + DIFF_V], axis=-1)
    q = rope_axial(q.reshape(B, L, DIFF_HEADS, 2, DIFF_DH), cos, sin)
    k = rope_axial(k.reshape(B, L, DIFF_HEADS, 2, DIFF_DH), cos, sin)
    v = v.reshape(B, L, DIFF_HEADS, 2 * DIFF_DH)
    kc, vc = jnp.split(hc @ w_in[:, DIFF_QK:2 * DIFF_QK + DIFF_V], [DIFF_QK], axis=-1)
    kc = kc.reshape(B, C, DIFF_HEADS, 2, DIFF_DH)
    vc = vc.reshape(B, C, DIFF_HEADS, 2 * DIFF_DH)
    k_all = jnp.concatenate([kc, k], axis=1)
    v_all = jnp.concatenate([vc, v], axis=1)
    nb = L // Q_BLOCK
    qb = jnp.swapaxes(q.reshape(B, nb, Q_BLOCK, DIFF_HEADS, 2, DIFF_DH), 0, 1)
    o = lax.map(lambda qq: diff_attend(qq, k_all, v_all, lam), qb)
    o = jnp.swapaxes(o, 0, 1).reshape(B, L, DIFF_HEADS, 2 * DIFF_DH)
    o = (rmsnorm(o, head_g) * (1.0 - lam_init)).reshape(B, L, DIFF_V)
    y = jnp.concatenate([o, fourier_mix(f)], axis=-1) @ w_out
    if not update_ctx:
        return y, None
    qc = (hc @ w_in[:, :DIFF_QK]).reshape(B, C, DIFF_HEADS, 2, DIFF_DH)
    fc = hc @ w_in[:, 2 * DIFF_QK + DIFF_V:]
    oc = diff_attend(qc, kc, vc, lam)
    oc = (rmsnorm(oc, head_g) * (1.0 - lam_init)).reshape(B, C, DIFF_V)
    yc = jnp.concatenate([oc, fourier_mix(fc)], axis=-1) @ w_out
    return y, yc


def sgu_mixer(h, w_in, b_in, ng, ws, bs, w_out):
    B, L, _ = h.shape
    z = jax.nn.gelu(h @ w_in + b_in)
    u, v = jnp.split(z, 2, axis=-1)
    v = rmsnorm(v, ng).reshape(B, L // CHUNK, CHUNK, SGU_GROUPS, SGU_GC)
    s = jnp.einsum('gpq,bnqgc->bnpgc', ws, v) + bs.T[None, None, :, :, None]
    return (u * s.reshape(B, L, SGU_W)) @ w_out


def peer_mix(h, wq, keys, eu, ev):
    shape = h.shape
    tb = h.reshape(-1, PEER_BLOCK, shape[-1])

    def block(t):
        q = (t @ wq).reshape(PEER_BLOCK, PEER_HEADS, 2, PEER_HALF)
        s = jnp.einsum('thsd,hskd->thsk', q, keys).astype(jnp.float32)
        s1, i1 = lax.top_k(s[:, :, 0], PEER_TOPK)
        s2, i2 = lax.top_k(s[:, :, 1], PEER_TOPK)
        cand = (s1[..., :, None] + s2[..., None, :]).reshape(PEER_BLOCK, PEER_HEADS, PEER_TOPK * PEER_TOPK)
        sc, ci = lax.top_k(cand, PEER_TOPK)
        idx = (jnp.take_along_axis(i1, ci // PEER_TOPK, axis=-1) * N_KEYS
               + jnp.take_along_axis(i2, ci % PEER_TOPK, axis=-1))
        gate = jax.nn.softmax(sc, axis=-1)
        u = jnp.take(eu, idx, axis=0)
        act = jax.nn.gelu(jnp.einsum('td,thkd->thk', t, u).astype(jnp.float32))
        vv = jnp.take(ev, idx, axis=0)
        return jnp.einsum('thk,thkd->td', (gate * act).astype(t.dtype), vv)

    return lax.map(block, tb).reshape(shape)


def setup_inputs(seed: int = 0) -> dict:
    key = jax.random.key(seed)
    ks = jax.random.split(key, 24)
    D = D_MODEL

    def nrm(k, shape, s):
        return jax.random.normal(k, shape, jnp.float32) * s

    return {
        'x': nrm(ks[0], (BATCH, SEQ, D), 1.0),
        'c': nrm(ks[1], (BATCH, D), 1.0),
        'ctx': nrm(ks[2], (BATCH, CTX_LEN, D), 1.0),
        'c_ctx': nrm(ks[3], (D,), 1.0),
        'ada_w': nrm(ks[4], (DEPTH, D, 6 * D), 0.5 * D ** -0.5),
        'ada_b': nrm(ks[5], (DEPTH, 6 * D), 0.02),
        'norm1_g': 1.0 + nrm(ks[6], (DEPTH, D), 0.02),
        'norm2_g': 1.0 + nrm(ks[7], (DEPTH, D), 0.02),
        'final_g': 1.0 + nrm(ks[8], (D,), 0.02),
        'even_w_in': nrm(ks[9], (N_EVEN, D, EVEN_IN), D ** -0.5),
        'even_w_out': nrm(ks[10], (N_EVEN, EVEN_MIX, D), EVEN_MIX ** -0.5),
        'diff_lambda': nrm(ks[11], (N_EVEN, 4, DIFF_DH), 0.1),
        'diff_norm_g': 1.0 + nrm(ks[12], (N_EVEN, 2 * DIFF_DH), 0.02),
        'odd_w_in': nrm(ks[13], (N_ODD, D, ODD_IN), D ** -0.5),
        'odd_b_in': nrm(ks[14], (N_ODD, ODD_IN), 0.02),
        'sgu_norm_g': 1.0 + nrm(ks[15], (N_ODD, SGU_W), 0.02),
        'sgu_w': nrm(ks[16], (N_ODD, SGU_GROUPS, CHUNK, CHUNK), 0.5 * CHUNK ** -0.5),
        'sgu_b': 1.0 + nrm(ks[17], (N_ODD, SGU_GROUPS, CHUNK), 0.02),
        'odd_w_out': nrm(ks[18], (N_ODD, SGU_W, D), SGU_W ** -0.5),
        'peer_wq': nrm(ks[19], (DEPTH, D, PEER_HEADS * PEER_DQ), D ** -0.5),
        'peer_keys': nrm(ks[20], (DEPTH, PEER_HEADS, 2, N_KEYS, PEER_HALF), PEER_HALF ** -0.5),
        'peer_u': nrm(ks[21], (DEPTH, N_EXPERTS, D), D ** -0.5),
        'peer_v': nrm(ks[22], (DEPTH, N_EXPERTS, D), 0.5),
    }


def reference(x, c, ctx, c_ctx, ada_w, ada_b, norm1_g, norm2_g, final_g,
              even_w_in, even_w_out, diff_lambda, diff_norm_g,
              odd_w_in, odd_b_in, sgu_norm_g, sgu_w, sgu_b, odd_w_out,
              peer_wq, peer_keys, peer_u, peer_v):
    L = x.shape[1]
    rows = L // GRID_W
    cos, sin = axial_rope_tables(rows, x.dtype)
    c_s = jax.nn.silu(c)
    cc_s = jax.nn.silu(c_ctx)
    for i in range(DEPTH):
        even = i % 2 == 0
        update_ctx = any(j % 2 == 0 for j in range(i + 1, DEPTH))
        sh1, sc1, g1, sh2, sc2, g2 = jnp.split((c_s @ ada_w[i] + ada_b[i])[:, None, :], 6, axis=-1)
        h = rmsnorm(x, norm1_g[i]) * (1 + sc1) + sh1
        if even or update_ctx:
            csh1, csc1, cg1, csh2, csc2, cg2 = jnp.split(cc_s @ ada_w[i] + ada_b[i], 6)
            hc = rmsnorm(ctx, norm1_g[i]) * (1 + csc1) + csh1
        if even:
            e = i // 2
            y, yc = even_mixer(h, hc, even_w_in[e], even_w_out[e], diff_lambda[e],
                               diff_norm_g[e], i + 1, cos, sin, update_ctx)
        else:
            od = i // 2
            y = sgu_mixer(h, odd_w_in[od], odd_b_in[od], sgu_norm_g[od], sgu_w[od], sgu_b[od], odd_w_out[od])
            yc = (sgu_mixer(hc, odd_w_in[od], odd_b_in[od], sgu_norm_g[od], sgu_w[od], sgu_b[od], odd_w_out[od])
                  if update_ctx else None)
        x = x + g1 * y
        x = x + g2 * peer_mix(rmsnorm(x, norm2_g[i]) * (1 + sc2) + sh2,
                              peer_wq[i], peer_keys[i], peer_u[i], peer_v[i])
        if update_ctx:
            ctx = ctx + cg1 * yc
            ctx = ctx + cg2 * peer_mix(rmsnorm(ctx, norm2_g[i]) * (1 + csc2) + csh2,
                                       peer_wq[i], peer_keys[i], peer_u[i], peer_v[i])
    return rmsnorm(x, final_g)
```

```python
import math
from contextlib import ExitStack
import numpy as np
import ml_dtypes
import concourse.bass as bass
import concourse.mybir as mybir
from concourse.bass_utils import run_bass_kernel_spmd

F32 = mybir.dt.float32; BF16 = mybir.dt.bfloat16; U32 = mybir.dt.uint32; I32 = mybir.dt.int32
AF = mybir.ActivationFunctionType; ALU = mybir.AluOpType; AX = mybir.AxisListType

D = 1024; NPOS = 4096; NCTX = 256; NKEY = NPOS + NCTX; OWN = 2048; NT = OWN // 128
EPS = 1e-6
NEXP = 16384


class Buf:
    __slots__ = ('ap', 'name', 'lastw', 'readers', 'sem', 'cnt', 'uid', 'ssem', 'scnt')
    _serial = [0]

    def __init__(self, ap, name):
        self.ap = ap; self.name = name; self.lastw = None; self.readers = {}; self.sem = None; self.cnt = 0; self.ssem = None; self.scnt = 0
        Buf._serial[0] += 1; self.uid = Buf._serial[0]

    def __getitem__(self, k):
        return self.ap[k]


class Prog:
    def __init__(self, nc, es):
        self.nc = nc; self.es = es
        self.eng = {'pe': nc.tensor, 'act': nc.scalar, 'dve': nc.vector, 'pool': nc.gpsimd, 'sp': nc.sync}
        self.esem = {e: es.enter_context(nc.semaphore('sem_' + e)) for e in self.eng}
        self.ecnt = {e: 0 for e in self.eng}
        self.waited = {}
        self.nsem = 0
        self.dmabufs = []
        self.storebufs = {}
        self.free_sems = []
        self.nb = 0

    def sb(self, shape, dt, name=None, es=None):
        self.nb += 1
        name = name or ('b%d' % self.nb)
        t = (es or self.es).enter_context(self.nc.sbuf_tensor(name + '_%d' % self.nb, list(shape), dt))
        return Buf(t, name)

    def ps(self, shape, dt, name=None, es=None):
        self.nb += 1
        name = name or ('p%d' % self.nb)
        t = (es or self.es).enter_context(self.nc.psum_tensor(name + '_%d' % self.nb, list(shape), dt))
        return Buf(t, name)

    def dram(self, name, shape, dt, kind="Internal"):
        t = self.nc.dram_tensor(name, list(shape), dt, kind=kind)
        return Buf(t.ap(), name)

    def _wait(self, e, ev):
        key = (e, ev[2])
        if self.waited.get(key, 0) >= ev[1]:
            return
        self.eng[e].wait_ge(ev[0], ev[1]); self.waited[key] = ev[1]

    def _deps(self, e, reads, writes):
        evs = []
        for b in reads:
            if b.lastw is not None: evs.append(b.lastw)
        for b in writes:
            if b.lastw is not None: evs.append(b.lastw)
            evs.extend(b.readers.values())
        for ev in evs:
            if e == 'pe' and ev[2] == 'pe': continue
            self._wait(e, ev)

    def op(self, e, fn, reads=(), writes=()):
        self._deps(e, reads, writes)
        ins = fn(self.eng[e])
        self.ecnt[e] += 1
        ins.then_inc(self.esem[e], 1)
        ev = (self.esem[e], self.ecnt[e], e)
        for b in writes: b.lastw = ev; b.readers = {}
        for b in reads:
            if b not in writes: b.readers[e] = ev
        return ins

    def _slot(self):
        if self.free_sems:
            return self.free_sems.pop()
        self.nsem += 1
        return [self.es.enter_context(self.nc.semaphore('dsem%d' % self.nsem)), 0, 'dsem%d' % self.nsem]

    def dma(self, q, out_b, out_ap, in_b, in_ap, fn=None, extra_reads=(), nowaw=False):
        if out_b.sem is None:
            out_b.sem = self._slot()
        slot = out_b.sem
        key = slot[2]
        saved = None
        if nowaw and out_b.lastw is not None and out_b.lastw[2] == key:
            saved = out_b.lastw; out_b.lastw = None
        self._deps(q, [in_b] + list(extra_reads), [out_b])
        if saved is not None:
            out_b.lastw = saved
        slot[1] += 1
        if fn is None:
            ins = self.eng[q].dma_start(out=out_ap, in_=in_ap)
        else:
            ins = fn(self.eng[q])
        ins.then_inc(slot[0], 16)
        ev = (slot[0], 16 * slot[1], key)
        out_b.lastw = ev
        if not nowaw: out_b.readers = {}
        in_b.readers[key] = ev
        for b in extra_reads: b.readers[key] = ev
        self.dmabufs.append(out_b)

    def store(self, q, out_ap, in_b, in_ap):
        self._deps(q, [in_b], [])
        if in_b.ssem is None:
            in_b.ssem = self._slot()
        slot = in_b.ssem
        slot[1] += 1
        self.eng[q].dma_start(out=out_ap, in_=in_ap).then_inc(slot[0], 16)
        in_b.readers[slot[2]] = (slot[0], 16 * slot[1], slot[2])
        self.storebufs[in_b.uid] = in_b

    def finish(self, bufs):
        for b in bufs:
            if b.lastw is not None: self._wait('sp', b.lastw)

    def barrier(self):
        evs = [(self.esem[e], self.ecnt[e], e) for e in self.eng if self.ecnt[e] > 0]
        slots = {}
        for b in self.dmabufs:
            if b.sem is not None:
                slots[b.sem[2]] = b.sem; b.sem = None
        for b in self.storebufs.values():
            if b.ssem is not None:
                slots[b.ssem[2]] = b.ssem; b.ssem = None
        self.dmabufs = []; self.storebufs = {}
        for sl in slots.values():
            if sl[1] > 0:
                evs.append((sl[0], 16 * sl[1], sl[2]))
        for e in self.eng:
            for ev in evs:
                if ev[2] != e: self._wait(e, ev)
        self.free_sems.extend(slots.values())


def make_ident(P, es, dt):
    idf = P.sb([128, 128], F32, 'identf', es=es)
    P.op('pool', lambda e: e.memset(idf[:], 0.0), [], [idf])
    P.op('pool', lambda e: e.affine_select(out=idf[:], in_=idf[:], pattern=[[-1, 128]], compare_op=ALU.not_equal,
                                           fill=1.0, base=0, channel_multiplier=1), [idf], [idf])
    if dt == F32:
        return idf
    idb = P.sb([128, 128], dt, 'identb', es=es)
    P.op('dve', lambda e: e.tensor_copy(out=idb[:], in_=idf[:]), [idf], [idb])
    return idb


def rms_rstd(P, src, ncol, ss, rstd, junk, srcb=()):
    P.op('act', lambda e: e.activation(out=junk[:, 0:ncol], in_=src, func=AF.Square), list(srcb), [junk])
    P.op('dve', lambda e: e.reduce_sum(out=ss[:], in_=junk[:, 0:ncol], axis=AX.X), [junk], [ss])
    P.op('act', lambda e: e.activation(out=ss[:], in_=ss[:], func=AF.Sqrt, scale=1.0 / ncol, bias=P.epsb[:]), [ss, P.epsb], [ss])
    P.op('dve', lambda e: e.reciprocal(out=rstd[:], in_=ss[:]), [ss], [rstd])


def phase_mod(P, io, l, mods, with_ctx):
    with ExitStack() as es:
        n_src = 2 if with_ctx else 1
        cT = P.sb([128, 2, 8], F32, 'cT', es=es)
        P.dma('sp', cT, cT[:, 0, :], io['cT'], io['cT'][:, :])
        lhs = []
        cs = P.sb([128, 2, 8], F32, 'cs', es=es)
        if with_ctx:
            cc = P.sb([128, 8], F32, 'cc', es=es)
            P.dma('sp', cc, cc[:], io['ccT'], io['ccT'][:, :])
            P.op('act', lambda e: e.activation(out=cs[:, 1, :], in_=cc[:], func=AF.Silu), [cc], [cs])
        P.op('act', lambda e: e.activation(out=cs[:, 0, :], in_=cT[:, 0, :], func=AF.Silu), [cT], [cs])
        for s in range(n_src):
            lb = P.sb([128, 8, 128], F32, 'lhsb', es=es)
            P.op('dve', lambda e: e.tensor_copy(out=lb[:], in_=cs[:, s, :].unsqueeze(2).to_broadcast([128, 8, 128])), [cs], [lb])
            lhs.append(lb)
        ones1 = P.sb([1, 128], F32, 'ones1', es=es)
        P.op('dve', lambda e: e.memset(ones1[:], 1.0), [], [ones1])
        bias = P.sb([1, 6144], F32, 'adab', es=es)
        P.dma('sp', bias, bias[:], io['ada_b'], io['ada_b'][l:l + 1, :])
        gb = [P.sb([128, 1024], F32, 'gbc', es=es) for _ in range(2)]
        P.dma('sp', gb[0], gb[0][:], io['norm1_g'], io['norm1_g'][l:l + 1, :].partition_broadcast(128))
        P.dma('sp', gb[1], gb[1][:], io['norm2_g'], io['norm2_g'][l:l + 1, :].partition_broadcast(128))
        wb = [P.sb([128, 8, 1024], F32, 'adaw', es=es) for _ in range(2)]
        pm = [P.ps([128, 512], F32, 'pm', es=es) for _ in range(4)]
        names = ['B1', 'A1', 'G1', 'B2', 'A2', 'G2']
        ip = 0
        for j in range(6):
            w = wb[j % 2]
            P.dma('sp', w, w[:], io['ada_w'], io['ada_w'][l, :, j * 1024:(j + 1) * 1024].rearrange("(kc p) n -> p kc n", p=128))
            for s in range(n_src):
                if s == 1 and j > 1:
                    continue
                dst = mods[names[j] + ('c' if s == 1 else '')]
                for half in range(2):
                    p_ = pm[ip % 4]; ip += 1
                    for kc in range(8):
                        P.op('pe', lambda e: e.matmul(p_[:], lhsT=lhs[s][:, kc, :], rhs=w[:, kc, half * 512:(half + 1) * 512],
                                                      start=(kc == 0), stop=False), [lhs[s], w], [p_])
                    P.op('pe', lambda e: e.matmul(p_[:], lhsT=ones1[0:1, :], rhs=bias[0:1, j * 1024 + half * 512: j * 1024 + (half + 1) * 512],
                                                  start=False, stop=True), [ones1, bias], [p_])
                    sl = slice(half * 512, (half + 1) * 512)
                    if names[j][0] == 'A':
                        g_ = gb[0] if j == 1 else gb[1]
                        P.op('dve', lambda e: e.scalar_tensor_tensor(out=dst[:, sl], in0=p_[:], scalar=1.0, in1=g_[:, sl],
                                                                     op0=ALU.add, op1=ALU.mult), [p_, g_], [dst])
                    else:
                        P.op('act', lambda e: e.copy(out=dst[:, sl], in_=p_[:]), [p_], [dst])
        P.barrier()


def norm_mod_tile(P, xt, A, B, rstd_bufs, tmp, hbf):
    ss, rstd, junk = rstd_bufs
    rms_rstd(P, xt[:], 1024, ss, rstd, junk, [xt])
    P.op('dve', lambda e: e.scalar_tensor_tensor(out=tmp[:], in0=xt[:], scalar=rstd[:, 0:1], in1=A[:],
                                                 op0=ALU.mult, op1=ALU.mult), [xt, rstd, A], [tmp])
    P.op('pool', lambda e: e.tensor_tensor(out=hbf[:], in0=tmp[:], in1=B[:], op=ALU.add), [tmp, B], [hbf])


def transpose_tile(P, src_bf, ptr, identb, dst, dst_ap):
    for kc in range(8):
        P.op('pe', lambda e: e.transpose(out=ptr[:, kc, :], in_=src_bf[:, kc * 128:(kc + 1) * 128], identity=identb[:]),
             [src_bf, identb], [ptr])
    P.op('act', lambda e: e.copy(out=dst_ap, in_=ptr[:]), [ptr], [dst])


def load_weight_bf16(P, es_phase, dram_b, dram_ap2d, ncols, name):
    wbf = P.sb([128, 8, ncols], BF16, name, es=es_phase)
    with ExitStack() as es:
        st = [P.sb([128, 8, 512], F32, 'wstage', es=es) for _ in range(2)]
        for i, c0 in enumerate(range(0, ncols, 512)):
            s_ = st[i % 2]
            P.dma('sp', s_, s_[:], dram_b, dram_ap2d[:, c0:c0 + 512].rearrange("(kc p) n -> p kc n", p=128))
            eng = 'dve' if i % 2 == 0 else 'pool'
            P.op(eng, lambda e: e.tensor_copy(out=wbf[:, :, c0:c0 + 512], in_=s_[:]), [s_], [wbf])
        P.barrier()
    return wbf


def phase_l0_proj(P, io, mods, sc):
    with ExitStack() as es:
        identb = make_ident(P, es, BF16)
        wbf = load_weight_bf16(P, es, io['w_in'], io['w_in'][:, :], 4096, 'w_in_bf')
        xt = [P.sb([128, 1024], F32, 'xt', es=es) for _ in range(2)]
        tmp = P.sb([128, 1024], F32, 'tmp', es=es)
        hbf = [P.sb([128, 1024], BF16, 'hbf', es=es) for _ in range(2)]
        ss = P.sb([128, 1], F32, 'ss', es=es); rstd = P.sb([128, 1], F32, 'rstd', es=es); junk = P.sb([128, 1024], F32, 'junk', es=es)
        hT = [P.sb([128, 8, 512], BF16, 'hT', es=es) for _ in range(2)]
        cosb = [P.sb([128, 512], F32, 'cos', es=es) for _ in range(2)]
        sinb = [P.sb([128, 512], F32, 'sin', es=es) for _ in range(2)]
        r1 = [P.sb([128, 512], F32, 'r1', es=es) for _ in range(2)]
        r2 = [P.sb([128, 512], F32, 'r2', es=es) for _ in range(2)]
        ko = [P.sb([128, 512], BF16, 'ko', es=es) for _ in range(3)]
        vo = [P.sb([128, 768], BF16, 'vo', es=es) for _ in range(2)]
        ptr = P.ps([128, 8, 128], BF16, 'ptr', es=es)
        pp = [P.ps([128, 512], F32, 'pp', es=es) for _ in range(4)]
        pv = [P.ps([128, 384], F32, 'pv', es=es) for _ in range(2)]
        cnt = {'pp': 0, 'ko': 0, 'x': 0, 'vo': 0}

        def proj_T(hT_, n, col0):
            p_ = pp[cnt['pp'] % 4]; cnt['pp'] += 1
            for kc in range(8):
                P.op('pe', lambda e: e.matmul(p_[:, 0:n], lhsT=wbf[:, kc, col0:col0 + 128], rhs=hT_[:, kc, 0:n],
                                              start=(kc == 0), stop=(kc == 7)), [wbf, hT_], [p_])
            return p_

        for g in range(9):
            n = 256 if g == 0 else 512
            ntile = n // 128
            T0 = 0 if g == 0 else NCTX + (g - 1) * 512
            pos0 = (g - 1) * 512
            hT_ = hT[g % 2]
            A = mods['A1c'] if g == 0 else mods['A1']
            B = mods['B1c'] if g == 0 else mods['B1']
            for t in range(ntile):
                x_ = xt[cnt['x'] % 2]; hb_ = hbf[cnt['x'] % 2]; cnt['x'] += 1
                if g == 0:
                    P.dma('sp', x_, x_[:], io['ctx'], io['ctx'][t * 128:(t + 1) * 128, :])
                else:
                    P.dma('sp', x_, x_[:], io['x'], io['x'][pos0 + t * 128: pos0 + (t + 1) * 128, :])
                norm_mod_tile(P, x_, A, B, (ss, rstd, junk), tmp, hb_)
                transpose_tile(P, hb_, ptr, identb, hT_, hT_[:, :, t * 128:(t + 1) * 128])
            own = g > 0 and pos0 < OWN
            if g > 0:
                cb = cosb[g % 2]; sb_ = sinb[g % 2]
                P.dma('sp', cb, cb[:], io['cosT'], io['cosT'][:, pos0:pos0 + 512])
                P.dma('sp', sb_, sb_[:], io['sinT'], io['sinT'][:, pos0:pos0 + 512])
            for which in (['k', 'q'] if own else ['k']):
                cbase = 768 if which == 'k' else 0
                pbase = 3328 if which == 'k' else 2560
                for c in range(6):
                    p1 = proj_T(hT_, n, cbase + c * 128)
                    o_ = ko[cnt['ko'] % 3]; cnt['ko'] += 1
                    if g == 0:
                        P.op('act', lambda e: e.copy(out=o_[:, 0:n], in_=p1[:, 0:n]), [p1], [o_])
                    else:
                        p2 = proj_T(hT_, n, pbase + c * 128)
                        a_ = r1[c % 2]; b_ = r2[c % 2]
                        P.op('dve', lambda e: e.tensor_tensor(out=a_[:], in0=p1[:], in1=cb[:], op=ALU.mult), [p1, cb], [a_])
                        P.op('dve', lambda e: e.tensor_tensor(out=b_[:], in0=p2[:], in1=sb_[:], op=ALU.mult), [p2, sb_], [b_])
                        P.op('pool', lambda e: e.tensor_tensor(out=o_[:], in0=a_[:], in1=b_[:], op=ALU.add), [a_, b_], [o_])
                    if which == 'k':
                        P.store('sp', sc['kT'][c, :, T0:T0 + n], o_, o_[:, 0:n])
                    else:
                        q0 = pos0
                        P.store('sp', sc['qT'][c, :, q0:q0 + 512], o_, o_[:, :])
            if g > 0:
                for c in range(2):
                    p1 = proj_T(hT_, n, 2304 + c * 128)
                    o_ = ko[cnt['ko'] % 3]; cnt['ko'] += 1
                    P.op('act', lambda e: e.copy(out=o_[:], in_=p1[:]), [p1], [o_])
                    P.store('sp', sc['fT'][c, :, pos0:pos0 + 512], o_, o_[:, :])
            for t in range(ntile):
                for hh in range(2):
                    for kc in range(8):
                        P.op('pe', lambda e: e.matmul(pv[hh][:], lhsT=hT_[:, kc, t * 128:(t + 1) * 128],
                                                      rhs=wbf[:, kc, 1536 + hh * 384: 1536 + (hh + 1) * 384],
                                                      start=(kc == 0), stop=(kc == 7)), [hT_, wbf], [pv[hh]])
                v_ = vo[cnt['vo'] % 2]; cnt['vo'] += 1
                P.op('act', lambda e: e.copy(out=v_[:, 0:384], in_=pv[0][:]), [pv[0]], [v_])
                P.op('dve', lambda e: e.tensor_copy(out=v_[:, 384:768], in_=pv[1][:]), [pv[1]], [v_])
                P.store('sp', sc['v'][T0 + t * 128: T0 + (t + 1) * 128, :], v_, v_[:])
        P.barrier()


def phase_l0_attn(P, io, sc, with_conv=True):
    lam_init = 0.8 - 0.6 * math.exp(-0.3 * 0.0)
    with ExitStack() as es:
        identb = make_ident(P, es, BF16)
        dl = P.sb([128, 256], F32, 'dl', es=es)
        P.dma('sp', dl, dl[:], io['dlam'], io['dlam'][0:1, :].partition_broadcast(128))
        lj = P.sb([128, 64], F32, 'lj', es=es)
        l2 = P.sb([128, 2], F32, 'l2', es=es)
        for i in range(2):
            P.op('dve', lambda e: e.tensor_tensor(out=lj[:], in0=dl[:, i * 128:i * 128 + 64], in1=dl[:, i * 128 + 64:i * 128 + 128], op=ALU.mult), [dl], [lj])
            P.op('dve', lambda e: e.reduce_sum(out=l2[:, i:i + 1], in_=lj[:], axis=AX.X), [lj], [l2])
        P.op('act', lambda e: e.activation(out=l2[:], in_=l2[:], func=AF.Exp), [l2], [l2])
        neglam = P.sb([128, 1], F32, 'neglam', es=es)
        P.op('dve', lambda e: e.tensor_tensor(out=neglam[:], in0=l2[:, 1:2], in1=l2[:, 0:1], op=ALU.subtract), [l2], [neglam])
        P.op('dve', lambda e: e.tensor_scalar(out=neglam[:], in0=neglam[:], scalar1=-lam_init, scalar2=None, op0=ALU.add), [neglam], [neglam])
        hg = P.sb([128, 128], F32, 'hg', es=es)
        P.dma('sp', hg, hg[:], io['head_g'], io['head_g'][0:1, :].partition_broadcast(128))
        P.op('dve', lambda e: e.tensor_scalar(out=hg[:], in0=hg[:], scalar1=1.0 - lam_init, scalar2=None, op0=ALU.mult), [hg], [hg])
        zer = P.sb([128, 512], BF16, 'zer', es=es)
        P.op('dve', lambda e: e.memset(zer[:], 0.0), [], [zer])

        kT = [P.sb([128, NKEY], BF16, 'kTc', es=es) for _ in range(2)]
        vx = [P.sb([128, 34, 130], BF16, 'vx', es=es) for _ in range(2)]
        qT = [P.sb([128, OWN], BF16, 'qTc', es=es) for _ in range(2)]
        mixc = [P.sb([128, OWN], BF16, 'mixc', es=es) for _ in range(2)]
        for v_ in vx:
            P.op('pool', lambda e: e.memset(v_[:, :, 128:130], 1.0), [], [v_])
        eT = [P.sb([128, 512], BF16, 'eT', es=es) for _ in range(3)]
        acc = [[P.ps([128, 512], F32, 'acc', es=es) for _ in range(2)] for _ in range(2)]
        sps = [P.ps([128, 512], F32, 'sps', es=es) for _ in range(3)]
        ptr = P.ps([128, 4, 128], BF16, 'ptr', es=es)
        sm = [P.sb([128, 8], F32, 'sm', es=es) for _ in range(2)]
        t1 = [P.sb([128, 128], F32, 't1', es=es) for _ in range(2)]
        dd = [P.sb([128, 128], F32, 'dd', es=es) for _ in range(2)]
        junk = P.sb([128, 128], F32, 'junk', es=es)
        obf = [P.sb([128, 128], BF16, 'obf', es=es) for _ in range(2)]
        accS = [P.sb([128, 4, 258], F32, 'accS', es=es) for _ in range(2)]
        neghalf = P.sb([128, 1], F32, 'neghalf', es=es)
        P.op('dve', lambda e: e.memset(neghalf[:], -0.5), [], [neghalf])
        trl = P.sb([128, 8], F32, 'trl', es=es)
        ie = 0
        cg = conv_gen(P, io, sc['uv'], es) if with_conv else None

        def load_head(c):
            P.dma('sp', kT[c % 2], kT[c % 2][:], sc['kT'], sc['kT'][c, :, :])
            P.dma('sp', vx[c % 2], vx[c % 2][:, :, 0:128], sc['v'], sc['v'][:, c * 128:(c + 1) * 128].rearrange("(kc p) e -> p kc e", p=128))
            P.dma('sp', qT[c % 2], qT[c % 2][:], sc['qT'], sc['qT'][c, :, :])

        sm4 = [P.sb([128, 8], F32, 'sm4', es=es) for _ in range(4)]
        t14 = [P.sb([128, 128], F32, 't14', es=es) for _ in range(4)]
        dd4 = [P.sb([128, 128], F32, 'dd4', es=es) for _ in range(4)]
        ob4 = [P.sb([128, 128], BF16, 'ob4', es=es) for _ in range(4)]

        def idle(n):
            for _ in range(n):
                yield

        def post(c, qg, aS, mx):
            yield from idle(3)
            for qt in range(4):
                o0 = (qt % 2) * 129; i0 = qt // 2; i1 = 2 + qt // 2
                s = sm4[qt]; t_ = t14[qt]; d_ = dd4[qt]
                P.op('dve', lambda e: e.reciprocal(out=s[:, 0:1], in_=aS[:, i0, o0 + 128:o0 + 129]), [aS], [s])
                P.op('dve', lambda e: e.reciprocal(out=s[:, 1:2], in_=aS[:, i1, o0 + 128:o0 + 129]), [aS], [s]); yield
                P.op('dve', lambda e: e.tensor_tensor(out=s[:, 2:3], in0=s[:, 1:2], in1=neglam[:], op=ALU.mult), [s, neglam], [s])
                P.op('dve', lambda e: e.tensor_scalar(out=t_[:], in0=aS[:, i1, o0:o0 + 128], scalar1=s[:, 2:3], scalar2=None, op0=ALU.mult), [aS, s], [t_]); yield
                P.op('dve', lambda e: e.scalar_tensor_tensor(out=d_[:], in0=aS[:, i0, o0:o0 + 128], scalar=s[:, 0:1], in1=t_[:],
                                                             op0=ALU.mult, op1=ALU.add), [aS, s, t_], [d_]); yield
                P.op('dve', lambda e: e.scalar_tensor_tensor(out=junk[:], in0=d_[:], scalar=1.0, in1=d_[:], op0=ALU.mult, op1=ALU.mult,
                                                             accum_out=s[:, 3:4]), [d_], [junk, s])
                P.op('dve', lambda e: e.memset(trl[:, 0:1], 0.0), [], [trl, s]); yield
            yield from idle(5)
            for qt in range(4):
                s = sm4[qt]
                P.op('act', lambda e: e.activation(out=s[:, 4:5], in_=s[:, 3:4], func=AF.Ln, scale=1.0 / 128, bias=P.epsb[:]), [s, P.epsb], [s])
                P.op('act', lambda e: e.activation(out=s[:, 5:6], in_=s[:, 4:5], func=AF.Exp, scale=-0.5), [s], [s]); yield
            yield from idle(5)
            for qt in range(4):
                s = sm4[qt]; d_ = dd4[qt]; ob = ob4[qt]
                P.op('dve', lambda e: e.scalar_tensor_tensor(out=ob[:], in0=d_[:], scalar=s[:, 5:6], in1=hg[:],
                                                             op0=ALU.mult, op1=ALU.mult), [d_, s, hg], [ob]); yield
            yield from idle(5)
            for qt in range(4):
                P.op('pe', lambda e: e.transpose(out=ptr[:, qt, :], in_=ob4[qt][:], identity=identb[:]), [ob4[qt], identb], [ptr])
            yield from idle(6)
            P.op('act', lambda e: e.copy(out=mx[:, qg * 512:(qg + 1) * 512], in_=ptr[:].rearrange("p a b -> p (a b)")), [ptr], [mx]); yield
            if qg == 3:
                P.store('sp', sc['mixT'][c, :, :], mx, mx[:]); yield

        def exhaust(gen):
            if gen is not None:
                for _ in gen:
                    pass

        pg = None; grp = 0
        load_head(0)
        for c in range(6):
            k_ = kT[c % 2]; v_ = vx[c % 2]; q_ = qT[c % 2]; mx = mixc[c % 2]
            if c + 1 < 6:
                load_head(c + 1)
            for qg in range(4):
                for m in range(2):
                    for hf in range(2):
                        P.op('pe', lambda e: e.matmul(acc[m][hf][:], lhsT=zer[:, 0:128], rhs=zer[:, :], start=True, stop=False,
                                                      skip_group_check=True), [zer], [acc[m][hf]])
                def pv(kc, m, e_):
                    for qt in range(4):
                        a_ = acc[m][qt // 2]
                        P.op('pe', lambda e: e.matmul(a_[:, (qt % 2) * 129:(qt % 2) * 129 + 129], lhsT=e_[:, qt * 128:(qt + 1) * 128],
                                                      rhs=v_[:, kc, 0:129], start=False, stop=(kc == 33), skip_group_check=True), [e_, v_], [a_])
                pend = None
                for kc in range(34):
                    for m in range(2):
                        s_ = sps[ie % 3]; e_ = eT[ie % 3]; ie += 1
                        P.op('pe', lambda e: e.matmul(s_[:], lhsT=k_[m * 64:(m + 1) * 64, kc * 128:(kc + 1) * 128],
                                                      rhs=q_[m * 64:(m + 1) * 64, qg * 512:(qg + 1) * 512], start=True, stop=True), [k_, q_], [s_])
                        P.op('act', lambda e: e.activation(out=e_[:], in_=s_[:], func=AF.Exp, scale=0.125), [s_], [e_])
                        if pend is not None:
                            pv(*pend)
                        pend = (kc, m, e_)
                        if cg is not None and ie % 3 != 0:
                            next(cg, None)
                        if pg is not None:
                            next(pg, None)
                pv(*pend)
                exhaust(pg)
                aS = accS[grp % 2]; grp += 1
                for m in range(2):
                    for hf in range(2):
                        if hf == 0:
                            P.op('act', lambda e: e.copy(out=aS[:, m * 2 + hf, :], in_=acc[m][hf][:, 0:258]), [acc[m][hf]], [aS])
                        else:
                            P.op('dve', lambda e: e.tensor_copy(out=aS[:, m * 2 + hf, :], in_=acc[m][hf][:, 0:258]), [acc[m][hf]], [aS])
                pg = post(c, qg, aS, mx)
        exhaust(pg)
        if cg is not None:
            for _ in cg:
                pass
        P.barrier()


def phase_l0_fourier(P, io, sc):
    with ExitStack() as es:
        c4 = P.sb([128, 2, 128], BF16, 'c4', es=es)
        P.dma('sp', c4, c4[:], io['c4s4'], io['c4s4'][:, :, :])
        fT = P.sb([128, 2, NPOS], BF16, 'fT', es=es)
        for c in range(2):
            P.dma('sp', fT, fT[:, c, :], sc['fT'], sc['fT'][c, :, :])
        X = P.sb([128, 32, 512], BF16, 'Xcs', es=es)
        px = [P.ps([128, 512], F32, 'px', es=es) for _ in range(2)]
        for nc_ in range(32):
            p_ = px[nc_ % 2]
            for cs in range(2):
                for c in range(2):
                    P.op('pe', lambda e: e.matmul(p_[:, cs * 256 + c * 128: cs * 256 + (c + 1) * 128],
                                                  lhsT=fT[:, c, nc_ * 128:(nc_ + 1) * 128], rhs=c4[:, cs, :], start=True, stop=True), [fT, c4], [p_])
            eng = 'act' if nc_ % 2 == 0 else 'dve'
            if eng == 'act':
                P.op('act', lambda e: e.copy(out=X[:, nc_, :], in_=p_[:]), [p_], [X])
            else:
                P.op('dve', lambda e: e.tensor_copy(out=X[:, nc_, :], in_=p_[:]), [p_], [X])
        tab = [P.sb([128, 32, 512], BF16, 'tab', es=es) for _ in range(2)]
        py = [P.ps([128, 512], F32, 'py', es=es) for _ in range(2)]
        yo = [P.sb([128, 512], BF16, 'yo', es=es) for _ in range(2)]
        for kg in range(4):
            for cs in range(2):
                for n8 in range(4):
                    P.dma('sp', tab[cs], tab[cs][:, n8 * 8:(n8 + 1) * 8, :], io['dft'],
                          io['dft'][cs, n8 * 1024:(n8 + 1) * 1024, kg * 512:(kg + 1) * 512].rearrange("(nc p) k -> p nc k", p=128), nowaw=True)
            for c in range(2):
                p_ = py[c]
                i = 0
                for cs in range(2):
                    for nc_ in range(32):
                        P.op('pe', lambda e: e.matmul(p_[:], lhsT=X[:, nc_, cs * 256 + c * 128: cs * 256 + (c + 1) * 128],
                                                      rhs=tab[cs][:, nc_, :], start=(i == 0), stop=(i == 63)), [X, tab[cs]], [p_])
                        i += 1
                o_ = yo[c]
                P.op('act', lambda e: e.activation(out=o_[:], in_=p_[:], func=AF.Identity, scale=1.0 / 512.0), [p_], [o_])
                P.store('sp', sc['mixT'][6 + c, :, kg * 512:(kg + 1) * 512], o_, o_[:])
        P.barrier()


def phase_wout_resid(P, io, mods, w_b, mix_b, xin_b, xin_ap, xout_b):
    with ExitStack() as es:
        wbf = load_weight_bf16(P, es, w_b, w_b[:, :], 1024, 'wout_bf')
        mt = [P.sb([128, 8, 128], BF16, 'mt', es=es) for _ in range(2)]
        xt = [P.sb([128, 1024], F32, 'xt', es=es) for _ in range(2)]
        yt = [P.sb([128, 1024], F32, 'yt', es=es) for _ in range(2)]
        py = [P.ps([128, 512], F32, 'py', es=es) for _ in range(4)]
        for t in range(NT):
            m_ = mt[t % 2]; x_ = xt[t % 2]; y_ = yt[t % 2]
            P.dma('sp', m_, m_[:], mix_b, mix_b[:, :, t * 128:(t + 1) * 128].rearrange("kc p t -> p kc t"))
            P.dma('sp', x_, x_[:], xin_b, xin_ap[t * 128:(t + 1) * 128, :])
            for hf in range(2):
                p_ = py[(2 * t + hf) % 4]
                for kc in range(8):
                    P.op('pe', lambda e: e.matmul(p_[:], lhsT=m_[:, kc, :], rhs=wbf[:, kc, hf * 512:(hf + 1) * 512],
                                                  start=(kc == 0), stop=(kc == 7)), [m_, wbf], [p_])
                sl = slice(hf * 512, (hf + 1) * 512)
                P.op('dve', lambda e: e.tensor_tensor(out=y_[:, sl], in0=p_[:], in1=mods['G1'][:, sl], op=ALU.mult), [p_, mods['G1']], [y_])
            P.op('pool', lambda e: e.tensor_tensor(out=y_[:], in0=y_[:], in1=x_[:], op=ALU.add), [y_, x_], [y_])
            P.store('sp', xout_b[t * 128:(t + 1) * 128, :], y_, y_[:])
        P.barrier()


def conv_gen(P, io, uv, es):
    R = 2
    su = [P.sb([128, R, 1024], F32, 'cvu', es=es) for _ in range(3)]
    sv = [P.sb([128, R, 1024], F32, 'cvv', es=es) for _ in range(2)]
    ob = [P.sb([128, R, 2048], BF16, 'cvo', es=es) for _ in range(2)]
    nblk = NEXP // (128 * R)
    blocks = [(l, b) for l in range(2) for b in range(nblk)]

    def loads(i):
        l, b = blocks[i]
        r0 = b * 128 * R
        P.dma('pool', su[i % 3], su[i % 3][:], io['peer_u'], io['peer_u'][l, r0:r0 + 128 * R, :].rearrange("(p r) d -> p r d", r=R))
        P.dma('pool', sv[i % 2], sv[i % 2][:], io['peer_v'], io['peer_v'][l, r0:r0 + 128 * R, :].rearrange("(p r) d -> p r d", r=R))

    loads(0)
    yield
    for i, (l, b) in enumerate(blocks):
        u_ = su[i % 3]; v_ = sv[i % 2]; o_ = ob[i % 2]
        r0 = b * 128 * R
        P.op('pool', lambda e: e.tensor_copy(out=o_[:, :, 1024:2048], in_=v_[:]), [v_], [o_])
        yield
        if i + 1 < len(blocks):
            loads(i + 1)
        yield
        P.op('dve', lambda e: e.tensor_copy(out=o_[:, :, 0:1024], in_=u_[:]), [u_], [o_])
        yield
        P.store('pool', uv[l * NEXP + r0: l * NEXP + r0 + 128 * R, :].rearrange("(p r) d -> p r d", r=R), o_, o_[:])
        yield


def phase_peer(P, io, l, mods, xin_b, xout_b, final, uv):
    with ExitStack() as es:
        identb = make_ident(P, es, BF16)
        wq = load_weight_bf16(P, es, io['peer_wq'], io['peer_wq'][l], 2048, 'wq_bf')
        kb = P.sb([128, 16, 128], BF16, 'kb', es=es)
        with ExitStack() as es2:
            kst = P.sb([128, 16, 128], F32, 'kst', es=es2)
            P.dma('sp', kst, kst[:], io['keysT'], io['keysT'][l])
            P.op('dve', lambda e: e.tensor_copy(out=kb[:], in_=kst[:]), [kst], [kb])
            P.barrier()
        cu = P.sb([128, 2], U32, 'cu', es=es)
        P.op('dve', lambda e: e.memset(cu[:, 0:1], 4), [], [cu])
        P.op('dve', lambda e: e.memset(cu[:, 1:2], 15), [], [cu])
        io16i = P.sb([128, 16], I32, 'io16i', es=es)
        P.op('pool', lambda e: e.iota(out=io16i[:], pattern=[[1, 16]], base=0, channel_multiplier=0), [], [io16i])
        io16 = P.sb([128, 16], F32, 'io16', es=es)
        P.op('dve', lambda e: e.tensor_copy(out=io16[:], in_=io16i[:]), [io16i], [io16])
        if final:
            fg = P.sb([128, 1024], F32, 'fg', es=es)
            P.dma('sp', fg, fg[:], io['final_g'], io['final_g'][0:1, :].partition_broadcast(128))
        xt = [P.sb([128, 1024], F32, 'xt', es=es) for _ in range(2)]
        tmp = P.sb([128, 1024], F32, 'tmp', es=es)
        tf = [P.sb([128, 1024], F32, 'tf', es=es) for _ in range(2)]
        tbs = [P.sb([128, 1024], BF16, 'tb', es=es) for _ in range(2)]
        prodb = [P.sb([128, 1024], BF16, 'prodb', es=es) for _ in range(4)]
        trailA = P.sb([128, 8], F32, 'trailA', es=es)
        ss = P.sb([128, 1], F32, 'ss', es=es); rstd = P.sb([128, 1], F32, 'rstd', es=es)
        ss2 = P.sb([128, 1], F32, 'ss2', es=es); rstd2 = P.sb([128, 1], F32, 'rstd2', es=es)
        pjunk = P.sb([128, 1024], BF16, 'pjunk', es=es)
        tT = P.sb([128, 8, 128], BF16, 'tT', es=es)
        qTb = P.sb([128, 16, 128], BF16, 'qTb', es=es)
        scs = P.sb([128, 16, 128], F32, 'scs', es=es)
        wk = P.sb([128, 2048], F32, 'wk', es=es)
        wk2 = P.sb([128, 2048], F32, 'wk2', es=es)
        tv = P.sb([128, 16, 16], F32, 'tv', es=es)
        ti = P.sb([128, 16, 16], U32, 'ti', es=es)
        tif = P.sb([128, 16, 16], F32, 'tif', es=es)
        cv = P.sb([128, 8, 16], F32, 'cv', es=es)
        ci = P.sb([128, 8, 16], U32, 'ci', es=es)
        au = P.sb([128, 8, 16], U32, 'au', es=es); bu = P.sb([128, 8, 16], U32, 'bu', es=es)
        af = P.sb([128, 8, 16], F32, 'af', es=es); bf = P.sb([128, 8, 16], F32, 'bf', es=es)
        sel = P.sb([128, 2, 8, 16], F32, 'sel', es=es)
        idxf = P.sb([128, 128], F32, 'idxf', es=es)
        idx = [P.sb([128, 128], U32, 'idx', es=es) for _ in range(2)]
        gt = [P.sb([128, 8, 16], F32, 'gt', es=es) for _ in range(2)]
        gs = P.sb([128, 8], F32, 'gs', es=es)
        trail = P.sb([128, 8], F32, 'trail', es=es)
        acc = [P.sb([128, 1024], F32, 'acc', es=es) for _ in range(2)]
        NB = 12 if final else 13
        gb = [P.sb([128, 2048], BF16, 'gb', es=es) for _ in range(NB)]
        dg = [P.sb([128, 128], BF16, 'dg', es=es) for _ in range(4)]
        dotsG = [P.sb([128, 4], F32, 'dotsG', es=es) for _ in range(3)]
        wgG = [P.sb([128, 4], F32, 'wgG', es=es) for _ in range(3)]
        ptr = P.ps([128, 8, 128], BF16, 'ptr', es=es)
        pq = [P.ps([128, 4, 128], F32, 'pq', es=es) for _ in range(2)]
        psc = [P.ps([128, 4, 128], F32, 'psc', es=es) for _ in range(2)]
        pacc = [P.ps([128, 512], F32, 'pacc', es=es) for _ in range(2)]

        scw_v = wk[:].rearrange("p (a b) -> p a b", b=128)
        candw_v = wk[:].rearrange("p (a b) -> p a b", b=256)
        cand_v = wk2[:].rearrange("p (a b) -> p a b", b=256)
        eq_v = wk2[:].rearrange("p (h a b) -> p h a b", a=16, b=16)

        def idle(n):
            for _ in range(n):
                yield

        def routing(t):
            x_ = xt[t % 2]; tf_ = tf[t % 2]; idx_ = idx[t % 2]; gt_ = gt[t % 2]; tb = tbs[t % 2]
            P.dma('sp', x_, x_[:], xin_b, xin_b[t * 128:(t + 1) * 128, :])
            yield from idle(10)
            P.op('dve', lambda e: e.scalar_tensor_tensor(out=tmp[:], in0=x_[:], scalar=1.0, in1=x_[:], op0=ALU.mult, op1=ALU.mult,
                                                         accum_out=ss[:]), [x_], [tmp, ss])
            P.op('dve', lambda e: e.memset(trail[:, 1:2], 0.0), [], [trail, ss]); yield
            yield
            P.op('act', lambda e: e.activation(out=ss[:], in_=ss[:], func=AF.Sqrt, scale=1.0 / 1024, bias=P.epsb[:]), [ss, P.epsb], [ss])
            yield from idle(3)
            P.op('dve', lambda e: e.reciprocal(out=rstd[:], in_=ss[:]), [ss], [rstd]); yield
            P.op('dve', lambda e: e.scalar_tensor_tensor(out=tmp[:], in0=x_[:], scalar=rstd[:, 0:1], in1=mods['A2'][:],
                                                         op0=ALU.mult, op1=ALU.mult), [x_, rstd, mods['A2']], [tmp]); yield
            P.op('dve', lambda e: e.tensor_tensor(out=tf_[:], in0=tmp[:], in1=mods['B2'][:], op=ALU.add), [tmp, mods['B2']], [tf_])
            yield from idle(2)
            P.op('act', lambda e: e.copy(out=tb[:], in_=tf_[:]), [tf_], [tb])
            yield from idle(2)
            transpose_tile(P, tb, ptr, identb, tT, tT[:])
            yield from idle(3)
            for g4 in range(4):
                p_ = pq[g4 % 2]
                for j in range(4):
                    hs = g4 * 4 + j
                    for kc in range(8):
                        P.op('pe', lambda e: e.matmul(p_[:, j, :], lhsT=wq[:, kc, hs * 128:(hs + 1) * 128], rhs=tT[:, kc, :],
                                                      start=(kc == 0), stop=(kc == 7)), [wq, tT], [p_])
                yield from idle(3)
                P.op('act', lambda e: e.copy(out=qTb[:, g4 * 4:(g4 + 1) * 4, :], in_=p_[:]), [p_], [qTb]); yield
            yield from idle(2)
            for g4 in range(4):
                p_ = psc[g4 % 2]
                for j in range(4):
                    hs = g4 * 4 + j
                    P.op('pe', lambda e: e.matmul(p_[:, j, :], lhsT=qTb[:, hs, :], rhs=kb[:, hs, :], start=True, stop=True), [qTb, kb], [p_])
                yield from idle(2)
                P.op('act', lambda e: e.copy(out=scs[:, g4 * 4:(g4 + 1) * 4, :], in_=p_[:]), [p_], [scs]); yield
            yield from idle(2)
            for hs in range(16):
                P.op('dve', lambda e: e.max(out=tv[:, hs, 0:8], in_=scs[:, hs, :]), [scs], [tv])
                P.op('dve', lambda e: e.max_index(out=ti[:, hs, 0:8], in_max=tv[:, hs, 0:8], in_values=scs[:, hs, :]), [scs, tv], [ti]); yield
                P.op('dve', lambda e: e.match_replace(out=scw_v[:, hs, :], in_to_replace=tv[:, hs, 0:8], in_values=scs[:, hs, :], imm_value=-1e30), [scs, tv], [wk])
                P.op('dve', lambda e: e.max(out=tv[:, hs, 8:16], in_=scw_v[:, hs, :]), [wk], [tv]); yield
                P.op('dve', lambda e: e.max_index(out=ti[:, hs, 8:16], in_max=tv[:, hs, 8:16], in_values=scw_v[:, hs, :]), [wk, tv], [ti]); yield
            P.op('dve', lambda e: e.tensor_copy(out=tif[:], in_=ti[:]), [ti], [tif])
            tvv = tv[:].rearrange("p (h s) k -> p h s k", s=2)
            tiv = tif[:].rearrange("p (h s) k -> p h s k", s=2)
            candv = wk2[:].rearrange("p (h a b) -> p h a b", a=16, b=16)
            P.op('dve', lambda e: e.tensor_tensor(out=candv, in0=tvv[:, :, 0, :].unsqueeze(3).to_broadcast([128, 8, 16, 16]),
                                                  in1=tvv[:, :, 1, :].unsqueeze(2).to_broadcast([128, 8, 16, 16]), op=ALU.add), [tv], [wk2]); yield
            for h in range(8):
                P.op('dve', lambda e: e.max(out=cv[:, h, 0:8], in_=cand_v[:, h, :]), [wk2], [cv])
                P.op('dve', lambda e: e.max_index(out=ci[:, h, 0:8], in_max=cv[:, h, 0:8], in_values=cand_v[:, h, :]), [wk2, cv], [ci]); yield
                P.op('dve', lambda e: e.match_replace(out=candw_v[:, h, :], in_to_replace=cv[:, h, 0:8], in_values=cand_v[:, h, :], imm_value=-1e30), [wk2, cv], [wk])
                P.op('dve', lambda e: e.max(out=cv[:, h, 8:16], in_=candw_v[:, h, :]), [wk], [cv]); yield
                P.op('dve', lambda e: e.max_index(out=ci[:, h, 8:16], in_max=cv[:, h, 8:16], in_values=candw_v[:, h, :]), [wk, cv], [ci]); yield
            P.op('dve', lambda e: e.tensor_scalar(out=au[:], in0=ci[:], scalar1=cu[:, 0:1], scalar2=None, op0=ALU.logical_shift_right), [ci, cu], [au])
            P.op('dve', lambda e: e.tensor_scalar(out=bu[:], in0=ci[:], scalar1=cu[:, 1:2], scalar2=None, op0=ALU.bitwise_and), [ci, cu], [bu]); yield
            P.op('dve', lambda e: e.tensor_copy(out=af[:], in_=au[:]), [au], [af])
            P.op('dve', lambda e: e.tensor_copy(out=bf[:], in_=bu[:]), [bu], [bf]); yield
            for s_, xf in ((0, af), (1, bf)):
                P.op('dve', lambda e: e.tensor_tensor(out=eq_v, in0=xf[:].unsqueeze(3).to_broadcast([128, 8, 16, 16]),
                                                      in1=io16[:].unsqueeze(1).unsqueeze(1).to_broadcast([128, 8, 16, 16]), op=ALU.is_equal), [xf, io16], [wk2]); yield
                P.op('dve', lambda e: e.tensor_tensor(out=eq_v, in0=eq_v, in1=tiv[:, :, s_, :].unsqueeze(2).to_broadcast([128, 8, 16, 16]), op=ALU.mult), [wk2, tif], [wk2]); yield
                P.op('dve', lambda e: e.tensor_reduce(out=sel[:, s_, :, :], in_=eq_v, axis=AX.X, op=ALU.add), [wk2], [sel]); yield
            P.op('dve', lambda e: e.scalar_tensor_tensor(out=idxf[:], in0=sel[:, 0, :, :].rearrange("p h k -> p (h k)"), scalar=128.0,
                                                         in1=sel[:, 1, :, :].rearrange("p h k -> p (h k)"), op0=ALU.mult, op1=ALU.add), [sel], [idxf])
            if l > 0:
                P.op('dve', lambda e: e.tensor_scalar(out=idxf[:], in0=idxf[:], scalar1=float(l * NEXP), scalar2=None, op0=ALU.add), [idxf], [idxf])
            P.op('dve', lambda e: e.tensor_copy(out=idx_[:], in_=idxf[:]), [idxf], [idx_]); yield
            P.op('dve', lambda e: e.tensor_tensor(out=gt_[:], in0=cv[:], in1=cv[:, :, 0:1].to_broadcast([128, 8, 16]), op=ALU.subtract), [cv], [gt_])
            P.op('act', lambda e: e.activation(out=gt_[:], in_=gt_[:], func=AF.Exp), [gt_], [gt_]); yield
            P.op('dve', lambda e: e.tensor_reduce(out=gs[:], in_=gt_[:], axis=AX.X, op=ALU.add), [gt_], [gs])
            P.op('dve', lambda e: e.reciprocal(out=gs[:], in_=gs[:]), [gs], [gs]); yield
            P.op('dve', lambda e: e.tensor_tensor(out=gt_[:], in0=gt_[:], in1=gs[:].unsqueeze(2).to_broadcast([128, 8, 16]), op=ALU.mult), [gt_, gs], [gt_]); yield

        def step(gen):
            if gen is not None:
                next(gen, None)

        def exhaust(gen):
            if gen is not None:
                for _ in gen:
                    pass

        exhaust(routing(0))
        ig = 0; iv = 0; ip = 0
        P.op('act', lambda e: e.copy(out=trailA[:], in_=trail[:]), [trail], [trailA])
        for t in range(NT):
            x_ = xt[t % 2]; tf_ = tf[t % 2]; idx_ = idx[t % 2]; gt_ = gt[t % 2]; acc_ = acc[t % 2]; tb_ = tbs[t % 2]
            nxt = routing(t + 1) if t + 1 < NT else None
            slots = {}
            for gi in range(33):
                if gi < 32:
                    dg_ = dotsG[gi % 3]
                    for s4 in range(4):
                        j = gi * 4 + s4
                        g_ = gb[ig % NB]; ig += 1
                        slots[j] = g_
                        P.dma('pool', g_, None, uv, None,
                              fn=lambda e: e.indirect_dma_start(out=g_[:], out_offset=None, in_=uv[:, :],
                                                                in_offset=bass.IndirectOffsetOnAxis(ap=idx_[:, j:j + 1], axis=0)), extra_reads=[idx_])
                        pr_ = prodb[ip % 4]; ip += 1
                        P.op('dve', lambda e: e.tensor_tensor(out=pr_[:], in0=g_[:, 0:1024], in1=tb_[:], op=ALU.mult), [g_, tb_], [pr_])
                        P.op('act', lambda e: e.activation(out=pjunk[:], in_=pr_[:], func=AF.Identity, accum_out=dg_[:, s4:s4 + 1]),
                             [pr_], ([dg_] if s4 == 0 else []))
                        step(nxt)
                        step(nxt)
                    P.op('act', lambda e: e.copy(out=trailA[:, 0:1], in_=trailA[:, 1:2]), [], [trailA, dg_])
                if gi >= 1:
                    gp = gi - 1
                    dp_ = dotsG[gp % 3]; w_ = wgG[gp % 3]
                    P.op('act', lambda e: e.activation(out=w_[:], in_=dp_[:], func=AF.Gelu_apprx_tanh), [dp_], [w_])
                    P.op('dve', lambda e: e.tensor_tensor(out=w_[:], in0=w_[:], in1=gt_[:].rearrange("p h k -> p (h k)")[:, gp * 4:gp * 4 + 4], op=ALU.mult), [w_, gt_], [w_])
                    for s4 in range(4):
                        j = gp * 4 + s4
                        g_ = slots.pop(j)
                        d_ = dg[iv % 4]; iv += 1
                        P.op('dve', lambda e: e.tensor_scalar(out=d_[:], in0=identb[:], scalar1=w_[:, s4:s4 + 1], scalar2=None, op0=ALU.mult), [identb, w_], [d_])
                        for hf in range(2):
                            P.op('pe', lambda e: e.matmul(pacc[hf][:], lhsT=d_[:], rhs=g_[:, 1024 + hf * 512:1024 + (hf + 1) * 512],
                                                          start=(j == 0), stop=(j == 127)), [d_, g_], [pacc[hf]])
            exhaust(nxt)
            for hf in range(2):
                sl = slice(hf * 512, (hf + 1) * 512)
                P.op('dve', lambda e: e.tensor_tensor(out=acc_[:, sl], in0=pacc[hf][:], in1=mods['G2'][:, sl], op=ALU.mult), [pacc[hf], mods['G2']], [acc_])
            P.op('dve', lambda e: e.tensor_tensor(out=acc_[:], in0=acc_[:], in1=x_[:], op=ALU.add), [acc_, x_], [acc_])
            if final:
                rms_rstd(P, acc_[:], 1024, ss2, rstd2, tmp, [acc_])
                P.op('dve', lambda e: e.scalar_tensor_tensor(out=acc_[:], in0=acc_[:], scalar=rstd2[:, 0:1], in1=fg[:],
                                                             op0=ALU.mult, op1=ALU.mult), [acc_, rstd2, fg], [acc_])
            P.store('sp', xout_b[t * 128:(t + 1) * 128, :], acc_, acc_[:])
        P.barrier()


def phase_l1_sgu(P, io, mods, xin_b, xout_b):
    with ExitStack() as es:
        identb = make_ident(P, es, BF16)
        win = load_weight_bf16(P, es, io['odd_w_in'], io['odd_w_in'][:, :], 2048, 'oddwin_bf')
        wout = load_weight_bf16(P, es, io['odd_w_out'], io['odd_w_out'][:, :], 1024, 'oddwout_bf')
        wst = P.sb([128, 8, 128], F32, 'wst', es=es)
        P.dma('sp', wst, wst[:], io['wsT'], io['wsT'][:, :, :])
        wsb = P.sb([128, 8, 128], BF16, 'wsb', es=es)
        P.op('dve', lambda e: e.tensor_copy(out=wsb[:], in_=wst[:]), [wst], [wsb])
        bsT = P.sb([128, 8], F32, 'bsT', es=es)
        P.dma('sp', bsT, bsT[:], io['bsT'], io['bsT'][:, :])
        binb = P.sb([128, 2048], F32, 'binb', es=es)
        P.dma('sp', binb, binb[:], io['odd_b_in'], io['odd_b_in'][0:1, :].partition_broadcast(128))
        ngb = P.sb([128, 1024], F32, 'ngb', es=es)
        P.dma('sp', ngb, ngb[:], io['sgu_norm_g'], io['sgu_norm_g'][0:1, :].partition_broadcast(128))
        xt = [P.sb([128, 1024], F32, 'xt', es=es) for _ in range(2)]
        tmp = P.sb([128, 1024], F32, 'tmp', es=es)
        hbf = P.sb([128, 1024], BF16, 'hbf', es=es)
        ss = P.sb([128, 1], F32, 'ss', es=es); rstd = P.sb([128, 1], F32, 'rstd', es=es); junk = P.sb([128, 1024], F32, 'junk', es=es)
        hT = P.sb([128, 8, 128], BF16, 'hT', es=es)
        zf = P.sb([128, 2048], F32, 'zf', es=es)
        vn = P.sb([128, 1024], BF16, 'vn', es=es)
        gat = P.sb([128, 1024], BF16, 'gat', es=es)
        gT = P.sb([128, 8, 128], BF16, 'gT', es=es)
        yt = [P.sb([128, 1024], F32, 'yt', es=es) for _ in range(2)]
        ptr = P.ps([128, 8, 128], BF16, 'ptr', es=es)
        pz = [P.ps([128, 512], F32, 'pz', es=es) for _ in range(4)]
        psg = [P.ps([128, 4, 128], F32, 'psg', es=es) for _ in range(2)]
        for t in range(NT):
            x_ = xt[t % 2]; y_ = yt[t % 2]
            P.dma('sp', x_, x_[:], xin_b, xin_b[t * 128:(t + 1) * 128, :])
            norm_mod_tile(P, x_, mods['A1'], mods['B1'], (ss, rstd, junk), tmp, hbf)
            transpose_tile(P, hbf, ptr, identb, hT, hT[:])
            for n4 in range(4):
                p_ = pz[n4]
                for kc in range(8):
                    P.op('pe', lambda e: e.matmul(p_[:], lhsT=hT[:, kc, :], rhs=win[:, kc, n4 * 512:(n4 + 1) * 512],
                                                  start=(kc == 0), stop=(kc == 7)), [hT, win], [p_])
                sl = slice(n4 * 512, (n4 + 1) * 512)
                P.op('dve', lambda e: e.tensor_tensor(out=zf[:, sl], in0=p_[:], in1=binb[:, sl], op=ALU.add), [p_, binb], [zf])
            P.op('act', lambda e: e.activation(out=zf[:], in_=zf[:], func=AF.Gelu_apprx_tanh), [zf], [zf])
            rms_rstd(P, zf[:, 1024:2048], 1024, ss, rstd, junk, [zf])
            P.op('dve', lambda e: e.scalar_tensor_tensor(out=vn[:], in0=zf[:, 1024:2048], scalar=rstd[:, 0:1], in1=ngb[:],
                                                         op0=ALU.mult, op1=ALU.mult), [zf, rstd, ngb], [vn])
            for g in range(8):
                p_ = psg[g // 4]
                P.op('pe', lambda e: e.matmul(p_[:, g % 4, :], lhsT=wsb[:, g, :], rhs=vn[:, g * 128:(g + 1) * 128], start=True, stop=True), [wsb, vn], [p_])
            for g in range(8):
                p_ = psg[g // 4]
                P.op('dve', lambda e: e.scalar_tensor_tensor(out=gat[:, g * 128:(g + 1) * 128], in0=p_[:, g % 4, :], scalar=bsT[:, g:g + 1],
                                                             in1=zf[:, g * 128:(g + 1) * 128], op0=ALU.add, op1=ALU.mult), [p_, bsT, zf], [gat])
            if t == 0 and getattr(P, 'dbg_sgu', None) is not None:
                dd_ = P.dbg_sgu
                P.dma('sp', dd_, dd_[0, :, :], zf, zf[:], nowaw=True)
                vf_ = P.sb([128, 2048], F32, 'dbgvf', es=es)
                P.op('dve', lambda e: e.tensor_copy(out=vf_[:, 0:1024], in_=vn[:]), [vn], [vf_])
                P.op('dve', lambda e: e.tensor_copy(out=vf_[:, 1024:2048], in_=gat[:]), [gat], [vf_])
                P.dma('sp', dd_, dd_[1, :, :], vf_, vf_[:], nowaw=True)
            transpose_tile(P, gat, ptr, identb, gT, gT[:])
            for hf in range(2):
                p_ = pz[hf]
                for kc in range(8):
                    P.op('pe', lambda e: e.matmul(p_[:], lhsT=gT[:, kc, :], rhs=wout[:, kc, hf * 512:(hf + 1) * 512],
                                                  start=(kc == 0), stop=(kc == 7)), [gT, wout], [p_])
                sl = slice(hf * 512, (hf + 1) * 512)
                P.op('dve', lambda e: e.tensor_tensor(out=y_[:, sl], in0=p_[:], in1=mods['G1'][:, sl], op=ALU.mult), [p_, mods['G1']], [y_])
            P.op('pool', lambda e: e.tensor_tensor(out=y_[:], in0=y_[:], in1=x_[:], op=ALU.add), [y_, x_], [y_])
            P.store('sp', xout_b[t * 128:(t + 1) * 128, :], y_, y_[:])
        P.barrier()


IN_SPECS = [
    ('x', [NPOS, D], F32), ('ctx', [NCTX, D], F32), ('cT', [128, 8], F32), ('ccT', [128, 8], F32),
    ('ada_w', [2, D, 6 * D], F32), ('ada_b', [2, 6 * D], F32), ('norm1_g', [2, D], F32), ('norm2_g', [2, D], F32),
    ('final_g', [1, D], F32), ('w_in', [D, 4096], F32), ('w_out', [D, D], F32), ('dlam', [1, 256], F32), ('head_g', [1, 128], F32),
    ('cosT', [128, NPOS], F32), ('sinT', [128, NPOS], F32), ('c4s4', [128, 2, 128], BF16), ('dft', [2, NPOS, OWN], BF16),
    ('odd_w_in', [D, 2048], F32), ('odd_b_in', [1, 2048], F32), ('sgu_norm_g', [1, D], F32), ('wsT', [128, 8, 128], F32),
    ('bsT', [128, 8], F32), ('odd_w_out', [D, D], F32),
    ('peer_wq', [2, D, 2048], F32), ('keysT', [2, 128, 16, 128], F32), ('peer_u', [2, NEXP, D], F32), ('peer_v', [2, NEXP, D], F32),
]
TEST_SMALL_PEER = False


def build(stop=None, debug_outs=()):
    nc = bass.Bass("TRN2", target_bir_lowering=False)
    with ExitStack() as es:
        P = Prog(nc, es)
        io = {}
        for name, shape, dt in IN_SPECS:
            if TEST_SMALL_PEER and name in ('peer_u', 'peer_v'):
                shape = [2, 128, D]
            io[name] = P.dram(name, shape, dt, kind="ExternalInput")
        out = P.dram('out', [OWN, D], F32, kind="ExternalOutput")
        def scr(name, shape, dt):
            return P.dram(name, shape, dt, kind=("ExternalOutput" if name in debug_outs else "Internal"))
        sc = {
            'kT': scr('kT_d', [6, 128, NKEY], BF16), 'v': scr('v_d', [NKEY, 768], BF16), 'qT': scr('qT_d', [6, 128, OWN], BF16),
            'fT': scr('fT_d', [2, 128, NPOS], BF16), 'mixT': scr('mixT_d', [8, 128, OWN], BF16),
            'uv': scr('uv_d', [2 * NEXP, 2 * D], BF16),
            'x1': scr('x1_d', [OWN, D], F32), 'x2': scr('x2_d', [OWN, D], F32), 'x3': scr('x3_d', [OWN, D], F32),
        }
        P.epsb = P.sb([128, 1], F32, 'epsb')
        P.op('dve', lambda e: e.memset(P.epsb[:], EPS), [], [P.epsb])
        mods = {n: P.sb([128, 1024], F32, 'mod_' + n) for n in ['A1', 'B1', 'G1', 'A2', 'B2', 'G2', 'A1c', 'B1c']}
        finals = [out]

        def run():
            phase_mod(P, io, 0, mods, True)
            if 'mods_d' in debug_outs:
                md = P.dram('mods_d', [8, 128, 1024], F32, kind="ExternalOutput")
                for i, n in enumerate(['A1', 'B1', 'G1', 'A2', 'B2', 'G2', 'A1c', 'B1c']):
                    P.dma('sp', md, md[i, :, :], mods[n], mods[n][:], nowaw=True)
                finals.append(md)
            if stop == 'mod0': return
            phase_l0_proj(P, io, mods, sc)
            if stop == 'proj': return
            phase_l0_attn(P, io, sc)
            if stop == 'attn': return
            phase_l0_fourier(P, io, sc)
            if stop == 'fourier': return
            phase_wout_resid(P, io, mods, io['w_out'], sc['mixT'], io['x'], io['x'][0:OWN, :], sc['x1'])
            if stop == 'x1': return
            phase_peer(P, io, 0, mods, sc['x1'], sc['x2'], False, sc['uv'])
            if stop == 'x2': return
            phase_mod(P, io, 1, mods, False)
            if 'sgu_d' in debug_outs:
                P.dbg_sgu = P.dram('sgu_d', [2, 128, 2048], F32, kind="ExternalOutput"); finals.append(P.dbg_sgu)
            phase_l1_sgu(P, io, mods, sc['x2'], sc['x3'])
            if stop == 'x3': return
            phase_peer(P, io, 1, mods, sc['x3'], out, True, sc['uv'])
        run()
        P.finish(list(sc.values()) + finals)
    return nc


def host_consts():
    pos = np.arange(NPOS)
    r = (pos // 64).astype(np.float32); col = (pos % 64).astype(np.float32)
    inv = (np.float32(10000.0) ** (-np.arange(16, dtype=np.float32) / np.float32(16))).astype(np.float32)
    ar = r[:, None] * inv; ac = col[:, None] * inv
    ang = np.concatenate([ar, ar, ac, ac], axis=-1).astype(np.float32)
    cos = np.cos(ang).astype(np.float32); sin = np.sin(ang).astype(np.float32)
    sign = np.concatenate([-np.ones(16), np.ones(16), -np.ones(16), np.ones(16)]).astype(np.float32)
    cosT = np.ascontiguousarray(np.concatenate([cos.T, cos.T], axis=0))
    sinT = np.ascontiguousarray(np.concatenate([(sin * sign).T, (sin * sign).T], axis=0))
    src = np.concatenate([np.arange(16, 32), np.arange(0, 16), np.arange(48, 64), np.arange(32, 48)])
    jj = np.arange(64)
    a4 = 2.0 * np.pi * np.outer(jj, jj) / 64.0
    c4 = np.zeros((128, 2, 128), np.float64)
    for b in range(2):
        c4[b * 64:(b + 1) * 64, 0, b * 64:(b + 1) * 64] = np.cos(a4)
        c4[b * 64:(b + 1) * 64, 1, b * 64:(b + 1) * 64] = -np.sin(a4)
    c4 = c4.astype(ml_dtypes.bfloat16)
    dfts = []
    for half in range(2):
        n = (np.arange(NPOS, dtype=np.int64) + half * OWN) % NPOS
        k = np.arange(half * OWN, (half + 1) * OWN, dtype=np.int64)
        ph = (np.outer(n, k) % NPOS).astype(np.float64) * (2.0 * np.pi / NPOS)
        dfts.append(np.stack([np.cos(ph), np.sin(ph)]).astype(ml_dtypes.bfloat16))
    return cosT, sinT, src, c4, dfts


def make_in_maps(inp, cores):
    cosT, sinT, src, c4, dfts = host_consts()
    w_in = inp['even_w_in'][0]
    permcols = np.concatenate([h * 64 + src for h in range(12)])
    w_ext = np.ascontiguousarray(np.concatenate([w_in, w_in[:, 0:768][:, permcols], w_in[:, 768:1536][:, permcols]], axis=1))
    keysT = np.ascontiguousarray(inp['peer_keys'].reshape(2, 16, 128, 128).transpose(0, 3, 1, 2))
    shared = {
        'ccT': np.ascontiguousarray(inp['c_ctx'].reshape(8, 128).T),
        'ada_w': inp['ada_w'], 'ada_b': inp['ada_b'], 'norm1_g': inp['norm1_g'], 'norm2_g': inp['norm2_g'],
        'final_g': inp['final_g'].reshape(1, D), 'w_in': w_ext, 'w_out': inp['even_w_out'][0],
        'dlam': inp['diff_lambda'].reshape(1, 256), 'head_g': inp['diff_norm_g'].reshape(1, 128),
        'cosT': cosT, 'sinT': sinT, 'c4s4': c4,
        'odd_w_in': inp['odd_w_in'][0], 'odd_b_in': inp['odd_b_in'].reshape(1, 2048), 'sgu_norm_g': inp['sgu_norm_g'].reshape(1, D),
        'wsT': np.ascontiguousarray(inp['sgu_w'][0].transpose(2, 0, 1)), 'bsT': np.ascontiguousarray(inp['sgu_b'][0].T),
        'odd_w_out': inp['odd_w_out'][0], 'peer_wq': inp['peer_wq'], 'keysT': keysT, 'peer_u': inp['peer_u'], 'peer_v': inp['peer_v'],
    }
    maps = []
    for core in cores:
        b, half = core // 2, core % 2
        m = dict(shared)
        order = (np.arange(NPOS) + half * OWN) % NPOS
        m['x'] = np.ascontiguousarray(inp['x'][b][order]); m['cosT'] = np.ascontiguousarray(cosT[:, order])
        m['sinT'] = np.ascontiguousarray(sinT[:, order]); m['ctx'] = np.ascontiguousarray(inp['ctx'][b])
        m['cT'] = np.ascontiguousarray(inp['c'][b].reshape(8, 128).T)
        m['dft'] = dfts[half]
        maps.append(m)
    return maps


def kernel(**inputs):
    inp = {k: np.asarray(v) for k, v in inputs.items()}
    nc = build()
    maps = make_in_maps(inp, list(range(8)))
    res = run_bass_kernel_spmd(nc, maps, core_ids=list(range(8)))
    out = np.zeros((4, NPOS, D), np.float32)
    for core in range(8):
        b, half = core // 2, core % 2
        out[b, half * OWN:(half + 1) * OWN, :] = res.results[core]['out']
    return out
```

```python
import math
from contextlib import ExitStack
import numpy as np
import ml_dtypes
import concourse.bass as bass
import concourse.mybir as mybir
from concourse.bass_utils import run_bass_kernel_spmd

F32 = mybir.dt.float32; BF16 = mybir.dt.bfloat16; U32 = mybir.dt.uint32; I32 = mybir.dt.int32
AF = mybir.ActivationFunctionType; ALU = mybir.AluOpType; AX = mybir.AxisListType

D = 1024; NPOS = 4096; NCTX = 256; NKEY = NPOS + NCTX; OWN = 2048; NT = OWN // 128
EPS = 1e-6
NEXP = 16384


class Buf:
    __slots__ = ('ap', 'name', 'lastw', 'readers', 'sem', 'cnt', 'uid', 'ssem', 'scnt')
    _serial = [0]

    def __init__(self, ap, name):
        self.ap = ap; self.name = name; self.lastw = None; self.readers = {}; self.sem = None; self.cnt = 0; self.ssem = None; self.scnt = 0
        Buf._serial[0] += 1; self.uid = Buf._serial[0]

    def __getitem__(self, k):
        return self.ap[k]


class Prog:
    def __init__(self, nc, es):
        self.nc = nc; self.es = es
        self.eng = {'pe': nc.tensor, 'act': nc.scalar, 'dve': nc.vector, 'pool': nc.gpsimd, 'sp': nc.sync}
        self.esem = {e: es.enter_context(nc.semaphore('sem_' + e)) for e in self.eng}
        self.ecnt = {e: 0 for e in self.eng}
        self.waited = {}
        self.nsem = 0
        self.dmabufs = []
        self.storebufs = {}
        self.free_sems = []
        self.nb = 0

    def sb(self, shape, dt, name=None, es=None):
        self.nb += 1
        name = name or ('b%d' % self.nb)
        t = (es or self.es).enter_context(self.nc.sbuf_tensor(name + '_%d' % self.nb, list(shape), dt))
        return Buf(t, name)

    def ps(self, shape, dt, name=None, es=None):
        self.nb += 1
        name = name or ('p%d' % self.nb)
        t = (es or self.es).enter_context(self.nc.psum_tensor(name + '_%d' % self.nb, list(shape), dt))
        return Buf(t, name)

    def dram(self, name, shape, dt, kind="Internal"):
        t = self.nc.dram_tensor(name, list(shape), dt, kind=kind)
        return Buf(t.ap(), name)

    def _wait(self, e, ev):
        key = (e, ev[2])
        if self.waited.get(key, 0) >= ev[1]:
            return
        self.eng[e].wait_ge(ev[0], ev[1]); self.waited[key] = ev[1]

    def _deps(self, e, reads, writes):
        evs = []
        for b in reads:
            if b.lastw is not None: evs.append(b.lastw)
        for b in writes:
            if b.lastw is not None: evs.append(b.lastw)
            evs.extend(b.readers.values())
        for ev in evs:
            if e == 'pe' and ev[2] == 'pe': continue
            self._wait(e, ev)

    def op(self, e, fn, reads=(), writes=()):
        self._deps(e, reads, writes)
        ins = fn(self.eng[e])
        self.ecnt[e] += 1
        ins.then_inc(self.esem[e], 1)
        ev = (self.esem[e], self.ecnt[e], e)
        for b in writes: b.lastw = ev; b.readers = {}
        for b in reads:
            if b not in writes: b.readers[e] = ev
        return ins

    def _slot(self):
        if self.free_sems:
            return self.free_sems.pop()
        self.nsem += 1
        return [self.es.enter_context(self.nc.semaphore('dsem%d' % self.nsem)), 0, 'dsem%d' % self.nsem]

    def dma(self, q, out_b, out_ap, in_b, in_ap, fn=None, extra_reads=(), nowaw=False):
        if out_b.sem is None:
            out_b.sem = self._slot()
        slot = out_b.sem
        key = slot[2]
        saved = None
        if nowaw and out_b.lastw is not None and out_b.lastw[2] == key:
            saved = out_b.lastw; out_b.lastw = None
        self._deps(q, [in_b] + list(extra_reads), [out_b])
        if saved is not None:
            out_b.lastw = saved
        slot[1] += 1
        if fn is None:
            ins = self.eng[q].dma_start(out=out_ap, in_=in_ap)
        else:
            ins = fn(self.eng[q])
        ins.then_inc(slot[0], 16)
        ev = (slot[0], 16 * slot[1], key)
        out_b.lastw = ev
        if not nowaw: out_b.readers = {}
        in_b.readers[key] = ev
        for b in extra_reads: b.readers[key] = ev
        self.dmabufs.append(out_b)

    def store(self, q, out_ap, in_b, in_ap):
        self._deps(q, [in_b], [])
        if in_b.ssem is None:
            in_b.ssem = self._slot()
        slot = in_b.ssem
        slot[1] += 1
        self.eng[q].dma_start(out=out_ap, in_=in_ap).then_inc(slot[0], 16)
        in_b.readers[slot[2]] = (slot[0], 16 * slot[1], slot[2])
        self.storebufs[in_b.uid] = in_b

    def finish(self, bufs):
        for b in bufs:
            if b.lastw is not None: self._wait('sp', b.lastw)

    def barrier(self):
        evs = [(self.esem[e], self.ecnt[e], e) for e in self.eng if self.ecnt[e] > 0]
        slots = {}
        for b in self.dmabufs:
            if b.sem is not None:
                slots[b.sem[2]] = b.sem; b.sem = None
        for b in self.storebufs.values():
            if b.ssem is not None:
                slots[b.ssem[2]] = b.ssem; b.ssem = None
        self.dmabufs = []; self.storebufs = {}
        for sl in slots.values():
            if sl[1] > 0:
                evs.append((sl[0], 16 * sl[1], sl[2]))
        for e in self.eng:
            for ev in evs:
                if ev[2] != e: self._wait(e, ev)
        self.free_sems.extend(slots.values())


def make_ident(P, es, dt):
    idf = P.sb([128, 128], F32, 'identf', es=es)
    P.op('pool', lambda e: e.memset(idf[:], 0.0), [], [idf])
    P.op('pool', lambda e: e.affine_select(out=idf[:], in_=idf[:], pattern=[[-1, 128]], compare_op=ALU.not_equal,
                                           fill=1.0, base=0, channel_multiplier=1), [idf], [idf])
    if dt == F32:
        return idf
    idb = P.sb([128, 128], dt, 'identb', es=es)
    P.op('dve', lambda e: e.tensor_copy(out=idb[:], in_=idf[:]), [idf], [idb])
    return idb


def rms_rstd(P, src, ncol, ss, rstd, junk, srcb=()):
    P.op('act', lambda e: e.activation(out=junk[:, 0:ncol], in_=src, func=AF.Square), list(srcb), [junk])
    P.op('dve', lambda e: e.reduce_sum(out=ss[:], in_=junk[:, 0:ncol], axis=AX.X), [junk], [ss])
    P.op('act', lambda e: e.activation(out=ss[:], in_=ss[:], func=AF.Sqrt, scale=1.0 / ncol, bias=P.epsb[:]), [ss, P.epsb], [ss])
    P.op('dve', lambda e: e.reciprocal(out=rstd[:], in_=ss[:]), [ss], [rstd])


def phase_mod(P, io, l, mods, with_ctx):
    with ExitStack() as es:
        n_src = 2 if with_ctx else 1
        cT = P.sb([128, 2, 8], F32, 'cT', es=es)
        P.dma('sp', cT, cT[:, 0, :], io['cT'], io['cT'][:, :])
        lhs = []
        cs = P.sb([128, 2, 8], F32, 'cs', es=es)
        if with_ctx:
            cc = P.sb([128, 8], F32, 'cc', es=es)
            P.dma('sp', cc, cc[:], io['ccT'], io['ccT'][:, :])
            P.op('act', lambda e: e.activation(out=cs[:, 1, :], in_=cc[:], func=AF.Silu), [cc], [cs])
        P.op('act', lambda e: e.activation(out=cs[:, 0, :], in_=cT[:, 0, :], func=AF.Silu), [cT], [cs])
        for s in range(n_src):
            lb = P.sb([128, 8, 128], F32, 'lhsb', es=es)
            P.op('dve', lambda e: e.tensor_copy(out=lb[:], in_=cs[:, s, :].unsqueeze(2).to_broadcast([128, 8, 128])), [cs], [lb])
            lhs.append(lb)
        ones1 = P.sb([1, 128], F32, 'ones1', es=es)
        P.op('dve', lambda e: e.memset(ones1[:], 1.0), [], [ones1])
        bias = P.sb([1, 6144], F32, 'adab', es=es)
        P.dma('sp', bias, bias[:], io['ada_b'], io['ada_b'][l:l + 1, :])
        gb = [P.sb([128, 1024], F32, 'gbc', es=es) for _ in range(2)]
        P.dma('sp', gb[0], gb[0][:], io['norm1_g'], io['norm1_g'][l:l + 1, :].partition_broadcast(128))
        P.dma('sp', gb[1], gb[1][:], io['norm2_g'], io['norm2_g'][l:l + 1, :].partition_broadcast(128))
        wb = [P.sb([128, 8, 1024], F32, 'adaw', es=es) for _ in range(2)]
        pm = [P.ps([128, 512], F32, 'pm', es=es) for _ in range(4)]
        names = ['B1', 'A1', 'G1', 'B2', 'A2', 'G2']
        ip = 0
        for j in range(6):
            w = wb[j % 2]
            P.dma('sp', w, w[:], io['ada_w'], io['ada_w'][l, :, j * 1024:(j + 1) * 1024].rearrange("(kc p) n -> p kc n", p=128))
            for s in range(n_src):
                if s == 1 and j > 1:
                    continue
                dst = mods[names[j] + ('c' if s == 1 else '')]
                for half in range(2):
                    p_ = pm[ip % 4]; ip += 1
                    for kc in range(8):
                        P.op('pe', lambda e: e.matmul(p_[:], lhsT=lhs[s][:, kc, :], rhs=w[:, kc, half * 512:(half + 1) * 512],
                                                      start=(kc == 0), stop=False), [lhs[s], w], [p_])
                    P.op('pe', lambda e: e.matmul(p_[:], lhsT=ones1[0:1, :], rhs=bias[0:1, j * 1024 + half * 512: j * 1024 + (half + 1) * 512],
                                                  start=False, stop=True), [ones1, bias], [p_])
                    sl = slice(half * 512, (half + 1) * 512)
                    if names[j][0] == 'A':
                        g_ = gb[0] if j == 1 else gb[1]
                        P.op('dve', lambda e: e.scalar_tensor_tensor(out=dst[:, sl], in0=p_[:], scalar=1.0, in1=g_[:, sl],
                                                                     op0=ALU.add, op1=ALU.mult), [p_, g_], [dst])
                    else:
                        P.op('act', lambda e: e.copy(out=dst[:, sl], in_=p_[:]), [p_], [dst])
        P.barrier()


def norm_mod_tile(P, xt, A, B, rstd_bufs, tmp, hbf):
    ss, rstd, junk = rstd_bufs
    rms_rstd(P, xt[:], 1024, ss, rstd, junk, [xt])
    P.op('dve', lambda e: e.scalar_tensor_tensor(out=tmp[:], in0=xt[:], scalar=rstd[:, 0:1], in1=A[:],
                                                 op0=ALU.mult, op1=ALU.mult), [xt, rstd, A], [tmp])
    P.op('pool', lambda e: e.tensor_tensor(out=hbf[:], in0=tmp[:], in1=B[:], op=ALU.add), [tmp, B], [hbf])


def transpose_tile(P, src_bf, ptr, identb, dst, dst_ap):
    for kc in range(8):
        P.op('pe', lambda e: e.transpose(out=ptr[:, kc, :], in_=src_bf[:, kc * 128:(kc + 1) * 128], identity=identb[:]),
             [src_bf, identb], [ptr])
    P.op('act', lambda e: e.copy(out=dst_ap, in_=ptr[:]), [ptr], [dst])


def load_weight_bf16(P, es_phase, dram_b, dram_ap2d, ncols, name):
    wbf = P.sb([128, 8, ncols], BF16, name, es=es_phase)
    with ExitStack() as es:
        st = [P.sb([128, 8, 512], F32, 'wstage', es=es) for _ in range(2)]
        for i, c0 in enumerate(range(0, ncols, 512)):
            s_ = st[i % 2]
            P.dma('sp', s_, s_[:], dram_b, dram_ap2d[:, c0:c0 + 512].rearrange("(kc p) n -> p kc n", p=128))
            eng = 'dve' if i % 2 == 0 else 'pool'
            P.op(eng, lambda e: e.tensor_copy(out=wbf[:, :, c0:c0 + 512], in_=s_[:]), [s_], [wbf])
        P.barrier()
    return wbf


def phase_l0_proj(P, io, mods, sc):
    with ExitStack() as es:
        identb = make_ident(P, es, BF16)
        wbf = load_weight_bf16(P, es, io['w_in'], io['w_in'][:, :], 4096, 'w_in_bf')
        xt = [P.sb([128, 1024], F32, 'xt', es=es) for _ in range(2)]
        tmp = P.sb([128, 1024], F32, 'tmp', es=es)
        hbf = [P.sb([128, 1024], BF16, 'hbf', es=es) for _ in range(2)]
        ss = P.sb([128, 1], F32, 'ss', es=es); rstd = P.sb([128, 1], F32, 'rstd', es=es); junk = P.sb([128, 1024], F32, 'junk', es=es)
        hT = [P.sb([128, 8, 512], BF16, 'hT', es=es) for _ in range(2)]
        cosb = [P.sb([128, 512], F32, 'cos', es=es) for _ in range(2)]
        sinb = [P.sb([128, 512], F32, 'sin', es=es) for _ in range(2)]
        r1 = [P.sb([128, 512], F32, 'r1', es=es) for _ in range(2)]
        r2 = [P.sb([128, 512], F32, 'r2', es=es) for _ in range(2)]
        ko = [P.sb([128, 512], BF16, 'ko', es=es) for _ in range(3)]
        vo = [P.sb([128, 768], BF16, 'vo', es=es) for _ in range(2)]
        ptr = P.ps([128, 8, 128], BF16, 'ptr', es=es)
        pp = [P.ps([128, 512], F32, 'pp', es=es) for _ in range(4)]
        pv = [P.ps([128, 384], F32, 'pv', es=es) for _ in range(2)]
        cnt = {'pp': 0, 'ko': 0, 'x': 0, 'vo': 0}

        def proj_T(hT_, n, col0):
            p_ = pp[cnt['pp'] % 4]; cnt['pp'] += 1
            for kc in range(8):
                P.op('pe', lambda e: e.matmul(p_[:, 0:n], lhsT=wbf[:, kc, col0:col0 + 128], rhs=hT_[:, kc, 0:n],
                                              start=(kc == 0), stop=(kc == 7)), [wbf, hT_], [p_])
            return p_

        for g in range(9):
            n = 256 if g == 0 else 512
            ntile = n // 128
            T0 = 0 if g == 0 else NCTX + (g - 1) * 512
            pos0 = (g - 1) * 512
            hT_ = hT[g % 2]
            A = mods['A1c'] if g == 0 else mods['A1']
            B = mods['B1c'] if g == 0 else mods['B1']
            for t in range(ntile):
                x_ = xt[cnt['x'] % 2]; hb_ = hbf[cnt['x'] % 2]; cnt['x'] += 1
                if g == 0:
                    P.dma('sp', x_, x_[:], io['ctx'], io['ctx'][t * 128:(t + 1) * 128, :])
                else:
                    P.dma('sp', x_, x_[:], io['x'], io['x'][pos0 + t * 128: pos0 + (t + 1) * 128, :])
                norm_mod_tile(P, x_, A, B, (ss, rstd, junk), tmp, hb_)
                transpose_tile(P, hb_, ptr, identb, hT_, hT_[:, :, t * 128:(t + 1) * 128])
            own = g > 0 and pos0 < OWN
            if g > 0:
                cb = cosb[g % 2]; sb_ = sinb[g % 2]
                P.dma('sp', cb, cb[:], io['cosT'], io['cosT'][:, pos0:pos0 + 512])
                P.dma('sp', sb_, sb_[:], io['sinT'], io['sinT'][:, pos0:pos0 + 512])
            for which in (['k', 'q'] if own else ['k']):
                cbase = 768 if which == 'k' else 0
                pbase = 3328 if which == 'k' else 2560
                for c in range(6):
                    p1 = proj_T(hT_, n, cbase + c * 128)
                    o_ = ko[cnt['ko'] % 3]; cnt['ko'] += 1
                    if g == 0:
                        P.op('act', lambda e: e.copy(out=o_[:, 0:n], in_=p1[:, 0:n]), [p1], [o_])
                    else:
                        p2 = proj_T(hT_, n, pbase + c * 128)
                        a_ = r1[c % 2]; b_ = r2[c % 2]
                        P.op('dve', lambda e: e.tensor_tensor(out=a_[:], in0=p1[:], in1=cb[:], op=ALU.mult), [p1, cb], [a_])
                        P.op('dve', lambda e: e.tensor_tensor(out=b_[:], in0=p2[:], in1=sb_[:], op=ALU.mult), [p2, sb_], [b_])
                        P.op('pool', lambda e: e.tensor_tensor(out=o_[:], in0=a_[:], in1=b_[:], op=ALU.add), [a_, b_], [o_])
                    if which == 'k':
                        P.store('sp', sc['kT'][c, :, T0:T0 + n], o_, o_[:, 0:n])
                    else:
                        q0 = pos0
                        P.store('sp', sc['qT'][c, :, q0:q0 + 512], o_, o_[:, :])
            if g > 0:
                for c in range(2):
                    p1 = proj_T(hT_, n, 2304 + c * 128)
                    o_ = ko[cnt['ko'] % 3]; cnt['ko'] += 1
                    P.op('act', lambda e: e.copy(out=o_[:], in_=p1[:]), [p1], [o_])
                    P.store('sp', sc['fT'][c, :, pos0:pos0 + 512], o_, o_[:, :])
            for t in range(ntile):
                for hh in range(2):
                    for kc in range(8):
                        P.op('pe', lambda e: e.matmul(pv[hh][:], lhsT=hT_[:, kc, t * 128:(t + 1) * 128],
                                                      rhs=wbf[:, kc, 1536 + hh * 384: 1536 + (hh + 1) * 384],
                                                      start=(kc == 0), stop=(kc == 7)), [hT_, wbf], [pv[hh]])
                v_ = vo[cnt['vo'] % 2]; cnt['vo'] += 1
                P.op('act', lambda e: e.copy(out=v_[:, 0:384], in_=pv[0][:]), [pv[0]], [v_])
                P.op('dve', lambda e: e.tensor_copy(out=v_[:, 384:768], in_=pv[1][:]), [pv[1]], [v_])
                P.store('sp', sc['v'][T0 + t * 128: T0 + (t + 1) * 128, :], v_, v_[:])
        P.barrier()


def phase_l0_attn(P, io, sc, with_conv=True):
    lam_init = 0.8 - 0.6 * math.exp(-0.3 * 0.0)
    with ExitStack() as es:
        identb = make_ident(P, es, BF16)
        dl = P.sb([128, 256], F32, 'dl', es=es)
        P.dma('sp', dl, dl[:], io['dlam'], io['dlam'][0:1, :].partition_broadcast(128))
        lj = P.sb([128, 64], F32, 'lj', es=es)
        l2 = P.sb([128, 2], F32, 'l2', es=es)
        for i in range(2):
            P.op('dve', lambda e: e.tensor_tensor(out=lj[:], in0=dl[:, i * 128:i * 128 + 64], in1=dl[:, i * 128 + 64:i * 128 + 128], op=ALU.mult), [dl], [lj])
            P.op('dve', lambda e: e.reduce_sum(out=l2[:, i:i + 1], in_=lj[:], axis=AX.X), [lj], [l2])
        P.op('act', lambda e: e.activation(out=l2[:], in_=l2[:], func=AF.Exp), [l2], [l2])
        neglam = P.sb([128, 1], F32, 'neglam', es=es)
        P.op('dve', lambda e: e.tensor_tensor(out=neglam[:], in0=l2[:, 1:2], in1=l2[:, 0:1], op=ALU.subtract), [l2], [neglam])
        P.op('dve', lambda e: e.tensor_scalar(out=neglam[:], in0=neglam[:], scalar1=-lam_init, scalar2=None, op0=ALU.add), [neglam], [neglam])
        hg = P.sb([128, 128], F32, 'hg', es=es)
        P.dma('sp', hg, hg[:], io['head_g'], io['head_g'][0:1, :].partition_broadcast(128))
        P.op('dve', lambda e: e.tensor_scalar(out=hg[:], in0=hg[:], scalar1=1.0 - lam_init, scalar2=None, op0=ALU.mult), [hg], [hg])
        zer = P.sb([128, 512], BF16, 'zer', es=es)
        P.op('dve', lambda e: e.memset(zer[:], 0.0), [], [zer])

        kT = [P.sb([128, NKEY], BF16, 'kTc', es=es) for _ in range(2)]
        vx = [P.sb([128, 34, 130], BF16, 'vx', es=es) for _ in range(2)]
        qT = [P.sb([128, OWN], BF16, 'qTc', es=es) for _ in range(2)]
        mixc = [P.sb([128, OWN], BF16, 'mixc', es=es) for _ in range(2)]
        for v_ in vx:
            P.op('pool', lambda e: e.memset(v_[:, :, 128:130], 1.0), [], [v_])
        eT = [P.sb([128, 512], BF16, 'eT', es=es) for _ in range(3)]
        acc = [[P.ps([128, 512], F32, 'acc', es=es) for _ in range(2)] for _ in range(2)]
        sps = [P.ps([128, 512], F32, 'sps', es=es) for _ in range(3)]
        ptr = P.ps([128, 4, 128], BF16, 'ptr', es=es)
        sm = [P.sb([128, 8], F32, 'sm', es=es) for _ in range(2)]
        t1 = [P.sb([128, 128], F32, 't1', es=es) for _ in range(2)]
        dd = [P.sb([128, 128], F32, 'dd', es=es) for _ in range(2)]
        junk = P.sb([128, 128], F32, 'junk', es=es)
        obf = [P.sb([128, 128], BF16, 'obf', es=es) for _ in range(2)]
        accS = [P.sb([128, 4, 258], F32, 'accS', es=es) for _ in range(2)]
        neghalf = P.sb([128, 1], F32, 'neghalf', es=es)
        P.op('dve', lambda e: e.memset(neghalf[:], -0.5), [], [neghalf])
        trl = P.sb([128, 8], F32, 'trl', es=es)
        ie = 0
        cg = conv_gen(P, io, sc['uv'], es, layers=(0,)) if with_conv else None

        def load_head(c):
            P.dma('sp', kT[c % 2], kT[c % 2][:], sc['kT'], sc['kT'][c, :, :])
            P.dma('sp', vx[c % 2], vx[c % 2][:, :, 0:128], sc['v'], sc['v'][:, c * 128:(c + 1) * 128].rearrange("(kc p) e -> p kc e", p=128))
            P.dma('sp', qT[c % 2], qT[c % 2][:], sc['qT'], sc['qT'][c, :, :])

        sm4 = [P.sb([128, 8], F32, 'sm4', es=es) for _ in range(4)]
        t14 = [P.sb([128, 128], F32, 't14', es=es) for _ in range(4)]
        dd4 = [P.sb([128, 128], F32, 'dd4', es=es) for _ in range(4)]
        ob4 = [P.sb([128, 128], BF16, 'ob4', es=es) for _ in range(4)]

        def idle(n):
            for _ in range(n):
                yield

        def post(c, qg, aS, mx):
            yield from idle(3)
            for qt in range(4):
                o0 = (qt % 2) * 129; i0 = qt // 2; i1 = 2 + qt // 2
                s = sm4[qt]; t_ = t14[qt]; d_ = dd4[qt]
                P.op('dve', lambda e: e.reciprocal(out=s[:, 0:1], in_=aS[:, i0, o0 + 128:o0 + 129]), [aS], [s])
                P.op('dve', lambda e: e.reciprocal(out=s[:, 1:2], in_=aS[:, i1, o0 + 128:o0 + 129]), [aS], [s]); yield
                P.op('dve', lambda e: e.tensor_tensor(out=s[:, 2:3], in0=s[:, 1:2], in1=neglam[:], op=ALU.mult), [s, neglam], [s])
                P.op('dve', lambda e: e.tensor_scalar(out=t_[:], in0=aS[:, i1, o0:o0 + 128], scalar1=s[:, 2:3], scalar2=None, op0=ALU.mult), [aS, s], [t_]); yield
                P.op('dve', lambda e: e.scalar_tensor_tensor(out=d_[:], in0=aS[:, i0, o0:o0 + 128], scalar=s[:, 0:1], in1=t_[:],
                                                             op0=ALU.mult, op1=ALU.add), [aS, s, t_], [d_]); yield
                P.op('dve', lambda e: e.scalar_tensor_tensor(out=junk[:], in0=d_[:], scalar=1.0, in1=d_[:], op0=ALU.mult, op1=ALU.mult,
                                                             accum_out=s[:, 3:4]), [d_], [junk, s])
                P.op('dve', lambda e: e.memset(trl[:, 0:1], 0.0), [], [trl, s]); yield
            yield from idle(5)
            for qt in range(4):
                s = sm4[qt]
                P.op('act', lambda e: e.activation(out=s[:, 4:5], in_=s[:, 3:4], func=AF.Ln, scale=1.0 / 128, bias=P.epsb[:]), [s, P.epsb], [s])
                P.op('act', lambda e: e.activation(out=s[:, 5:6], in_=s[:, 4:5], func=AF.Exp, scale=-0.5), [s], [s]); yield
            yield from idle(5)
            for qt in range(4):
                s = sm4[qt]; d_ = dd4[qt]; ob = ob4[qt]
                P.op('dve', lambda e: e.scalar_tensor_tensor(out=ob[:], in0=d_[:], scalar=s[:, 5:6], in1=hg[:],
                                                             op0=ALU.mult, op1=ALU.mult), [d_, s, hg], [ob]); yield
            yield from idle(5)
            for qt in range(4):
                P.op('pe', lambda e: e.transpose(out=ptr[:, qt, :], in_=ob4[qt][:], identity=identb[:]), [ob4[qt], identb], [ptr])
            yield from idle(6)
            P.op('act', lambda e: e.copy(out=mx[:, qg * 512:(qg + 1) * 512], in_=ptr[:].rearrange("p a b -> p (a b)")), [ptr], [mx]); yield
            if qg == 3:
                P.store('sp', sc['mixT'][c, :, :], mx, mx[:]); yield

        def exhaust(gen):
            if gen is not None:
                for _ in gen:
                    pass

        pg = None; grp = 0
        load_head(0)
        for c in range(6):
            k_ = kT[c % 2]; v_ = vx[c % 2]; q_ = qT[c % 2]; mx = mixc[c % 2]
            if c + 1 < 6:
                load_head(c + 1)
            for qg in range(4):
                for m in range(2):
                    for hf in range(2):
                        P.op('pe', lambda e: e.matmul(acc[m][hf][:], lhsT=zer[:, 0:128], rhs=zer[:, :], start=True, stop=False,
                                                      skip_group_check=True), [zer], [acc[m][hf]])
                def pv(kc, m, e_):
                    for qt in range(4):
                        a_ = acc[m][qt // 2]
                        P.op('pe', lambda e: e.matmul(a_[:, (qt % 2) * 129:(qt % 2) * 129 + 129], lhsT=e_[:, qt * 128:(qt + 1) * 128],
                                                      rhs=v_[:, kc, 0:129], start=False, stop=(kc == 33), skip_group_check=True), [e_, v_], [a_])
                pend = None
                for kc in range(34):
                    for m in range(2):
                        s_ = sps[ie % 3]; e_ = eT[ie % 3]; ie += 1
                        P.op('pe', lambda e: e.matmul(s_[:], lhsT=k_[m * 64:(m + 1) * 64, kc * 128:(kc + 1) * 128],
                                                      rhs=q_[m * 64:(m + 1) * 64, qg * 512:(qg + 1) * 512], start=True, stop=True), [k_, q_], [s_])
                        P.op('act', lambda e: e.activation(out=e_[:], in_=s_[:], func=AF.Exp, scale=0.125), [s_], [e_])
                        if pend is not None:
                            pv(*pend)
                        pend = (kc, m, e_)
                        if cg is not None and ie % 3 == 0:
                            next(cg, None)
                        if pg is not None:
                            next(pg, None)
                pv(*pend)
                exhaust(pg)
                aS = accS[grp % 2]; grp += 1
                for m in range(2):
                    for hf in range(2):
                        if hf == 0:
                            P.op('act', lambda e: e.copy(out=aS[:, m * 2 + hf, :], in_=acc[m][hf][:, 0:258]), [acc[m][hf]], [aS])
                        else:
                            P.op('dve', lambda e: e.tensor_copy(out=aS[:, m * 2 + hf, :], in_=acc[m][hf][:, 0:258]), [acc[m][hf]], [aS])
                pg = post(c, qg, aS, mx)
        exhaust(pg)
        if cg is not None:
            for _ in cg:
                pass
        P.barrier()


def phase_l0_fourier(P, io, sc):
    with ExitStack() as es:
        c4 = P.sb([128, 2, 128], BF16, 'c4', es=es)
        P.dma('sp', c4, c4[:], io['c4s4'], io['c4s4'][:, :, :])
        fT = P.sb([128, 2, NPOS], BF16, 'fT', es=es)
        for c in range(2):
            P.dma('sp', fT, fT[:, c, :], sc['fT'], sc['fT'][c, :, :])
        X = P.sb([128, 32, 512], BF16, 'Xcs', es=es)
        px = [P.ps([128, 512], F32, 'px', es=es) for _ in range(2)]
        for nc_ in range(32):
            p_ = px[nc_ % 2]
            for cs in range(2):
                for c in range(2):
                    P.op('pe', lambda e: e.matmul(p_[:, cs * 256 + c * 128: cs * 256 + (c + 1) * 128],
                                                  lhsT=fT[:, c, nc_ * 128:(nc_ + 1) * 128], rhs=c4[:, cs, :], start=True, stop=True), [fT, c4], [p_])
            eng = 'act' if nc_ % 2 == 0 else 'dve'
            if eng == 'act':
                P.op('act', lambda e: e.copy(out=X[:, nc_, :], in_=p_[:]), [p_], [X])
            else:
                P.op('dve', lambda e: e.tensor_copy(out=X[:, nc_, :], in_=p_[:]), [p_], [X])
        tab = [P.sb([128, 32, 512], BF16, 'tab', es=es) for _ in range(2)]
        py = [P.ps([128, 512], F32, 'py', es=es) for _ in range(2)]
        yo = [P.sb([128, 512], BF16, 'yo', es=es) for _ in range(2)]
        for kg in range(4):
            for cs in range(2):
                for n8 in range(4):
                    P.dma('sp', tab[cs], tab[cs][:, n8 * 8:(n8 + 1) * 8, :], io['dft'],
                          io['dft'][cs, n8 * 1024:(n8 + 1) * 1024, kg * 512:(kg + 1) * 512].rearrange("(nc p) k -> p nc k", p=128), nowaw=True)
            for c in range(2):
                p_ = py[c]
                i = 0
                for cs in range(2):
                    for nc_ in range(32):
                        P.op('pe', lambda e: e.matmul(p_[:], lhsT=X[:, nc_, cs * 256 + c * 128: cs * 256 + (c + 1) * 128],
                                                      rhs=tab[cs][:, nc_, :], start=(i == 0), stop=(i == 63)), [X, tab[cs]], [p_])
                        i += 1
                o_ = yo[c]
                P.op('act', lambda e: e.activation(out=o_[:], in_=p_[:], func=AF.Identity, scale=1.0 / 512.0), [p_], [o_])
                P.store('sp', sc['mixT'][6 + c, :, kg * 512:(kg + 1) * 512], o_, o_[:])
        P.barrier()


def phase_wout_resid(P, io, mods, w_b, mix_b, xin_b, xin_ap, xout_b):
    with ExitStack() as es:
        wbf = load_weight_bf16(P, es, w_b, w_b[:, :], 1024, 'wout_bf')
        mt = [P.sb([128, 8, 128], BF16, 'mt', es=es) for _ in range(2)]
        xt = [P.sb([128, 1024], F32, 'xt', es=es) for _ in range(2)]
        yt = [P.sb([128, 1024], F32, 'yt', es=es) for _ in range(2)]
        py = [P.ps([128, 512], F32, 'py', es=es) for _ in range(4)]
        for t in range(NT):
            m_ = mt[t % 2]; x_ = xt[t % 2]; y_ = yt[t % 2]
            P.dma('sp', m_, m_[:], mix_b, mix_b[:, :, t * 128:(t + 1) * 128].rearrange("kc p t -> p kc t"))
            P.dma('sp', x_, x_[:], xin_b, xin_ap[t * 128:(t + 1) * 128, :])
            for hf in range(2):
                p_ = py[(2 * t + hf) % 4]
                for kc in range(8):
                    P.op('pe', lambda e: e.matmul(p_[:], lhsT=m_[:, kc, :], rhs=wbf[:, kc, hf * 512:(hf + 1) * 512],
                                                  start=(kc == 0), stop=(kc == 7)), [m_, wbf], [p_])
                sl = slice(hf * 512, (hf + 1) * 512)
                P.op('dve', lambda e: e.tensor_tensor(out=y_[:, sl], in0=p_[:], in1=mods['G1'][:, sl], op=ALU.mult), [p_, mods['G1']], [y_])
            P.op('pool', lambda e: e.tensor_tensor(out=y_[:], in0=y_[:], in1=x_[:], op=ALU.add), [y_, x_], [y_])
            P.store('sp', xout_b[t * 128:(t + 1) * 128, :], y_, y_[:])
        P.barrier()


def conv_gen(P, io, uv, es, layers=(0, 1), R=4):
    su = [P.sb([128, R, 1024], F32, 'cvu', es=es) for _ in range(2)]
    sv = [P.sb([128, R, 1024], F32, 'cvv', es=es) for _ in range(2)]
    ob = [P.sb([128, R, 2048], BF16, 'cvo', es=es) for _ in range(2)]
    nblk = NEXP // (128 * R)
    i = 0
    for l in layers:
        for b in range(nblk):
            u_ = su[i % 2]; v_ = sv[i % 2]; o_ = ob[i % 2]; i += 1
            r0 = b * 128 * R
            P.dma('pool', u_, u_[:], io['peer_u'], io['peer_u'][l, r0:r0 + 128 * R, :].rearrange("(p r) d -> p r d", r=R))
            P.dma('pool', v_, v_[:], io['peer_v'], io['peer_v'][l, r0:r0 + 128 * R, :].rearrange("(p r) d -> p r d", r=R))
            yield
            P.op('pool', lambda e: e.tensor_copy(out=o_[:, :, 0:1024], in_=u_[:]), [u_], [o_])
            yield
            P.op('pool', lambda e: e.tensor_copy(out=o_[:, :, 1024:2048], in_=v_[:]), [v_], [o_])
            yield
            P.store('pool', uv[l * NEXP + r0: l * NEXP + r0 + 128 * R, :].rearrange("(p r) d -> p r d", r=R), o_, o_[:])
            yield


def phase_peer(P, io, l, mods, xin_b, xout_b, final, uv):
    with ExitStack() as es:
        identb = make_ident(P, es, BF16)
        wq = load_weight_bf16(P, es, io['peer_wq'], io['peer_wq'][l], 2048, 'wq_bf')
        kb = P.sb([128, 16, 128], BF16, 'kb', es=es)
        with ExitStack() as es2:
            kst = P.sb([128, 16, 128], F32, 'kst', es=es2)
            P.dma('sp', kst, kst[:], io['keysT'], io['keysT'][l])
            P.op('dve', lambda e: e.tensor_copy(out=kb[:], in_=kst[:]), [kst], [kb])
            P.barrier()
        cu = P.sb([128, 2], U32, 'cu', es=es)
        P.op('dve', lambda e: e.memset(cu[:, 0:1], 4), [], [cu])
        P.op('dve', lambda e: e.memset(cu[:, 1:2], 15), [], [cu])
        io16i = P.sb([128, 16], I32, 'io16i', es=es)
        P.op('pool', lambda e: e.iota(out=io16i[:], pattern=[[1, 16]], base=0, channel_multiplier=0), [], [io16i])
        io16 = P.sb([128, 16], F32, 'io16', es=es)
        P.op('dve', lambda e: e.tensor_copy(out=io16[:], in_=io16i[:]), [io16i], [io16])
        if final:
            fg = P.sb([128, 1024], F32, 'fg', es=es)
            P.dma('sp', fg, fg[:], io['final_g'], io['final_g'][0:1, :].partition_broadcast(128))
        xt = [P.sb([128, 1024], F32, 'xt', es=es) for _ in range(2)]
        tmp = P.sb([128, 1024], F32, 'tmp', es=es)
        tf = [P.sb([128, 1024], F32, 'tf', es=es) for _ in range(2)]
        tbs = [P.sb([128, 1024], BF16, 'tb', es=es) for _ in range(2)]
        prodb = [P.sb([128, 1024], BF16, 'prodb', es=es) for _ in range(4)]
        trailA = P.sb([128, 8], F32, 'trailA', es=es)
        ss = P.sb([128, 1], F32, 'ss', es=es); rstd = P.sb([128, 1], F32, 'rstd', es=es)
        ss2 = P.sb([128, 1], F32, 'ss2', es=es); rstd2 = P.sb([128, 1], F32, 'rstd2', es=es)
        pjunk = P.sb([128, 1024], BF16, 'pjunk', es=es)
        tT = P.sb([128, 8, 128], BF16, 'tT', es=es)
        qTb = P.sb([128, 16, 128], BF16, 'qTb', es=es)
        scs = P.sb([128, 16, 128], F32, 'scs', es=es)
        wk = P.sb([128, 2048], F32, 'wk', es=es)
        wk2 = P.sb([128, 2048], F32, 'wk2', es=es)
        tv = P.sb([128, 16, 16], F32, 'tv', es=es)
        ti = P.sb([128, 16, 16], U32, 'ti', es=es)
        tif = P.sb([128, 16, 16], F32, 'tif', es=es)
        cv = P.sb([128, 8, 16], F32, 'cv', es=es)
        ci = P.sb([128, 8, 16], U32, 'ci', es=es)
        au = P.sb([128, 8, 16], U32, 'au', es=es); bu = P.sb([128, 8, 16], U32, 'bu', es=es)
        af = P.sb([128, 8, 16], F32, 'af', es=es); bf = P.sb([128, 8, 16], F32, 'bf', es=es)
        sel = P.sb([128, 2, 8, 16], F32, 'sel', es=es)
        idxf = P.sb([128, 128], F32, 'idxf', es=es)
        idx = [P.sb([128, 128], U32, 'idx', es=es) for _ in range(2)]
        gt = [P.sb([128, 8, 16], F32, 'gt', es=es) for _ in range(2)]
        gs = P.sb([128, 8], F32, 'gs', es=es)
        trail = P.sb([128, 8], F32, 'trail', es=es)
        acc = [P.sb([128, 1024], F32, 'acc', es=es) for _ in range(2)]
        NB = 12 if final else 13
        gb = [P.sb([128, 2048], BF16, 'gb', es=es) for _ in range(NB)]
        dg = [P.sb([128, 128], BF16, 'dg', es=es) for _ in range(4)]
        dotsG = [P.sb([128, 4], F32, 'dotsG', es=es) for _ in range(3)]
        wgG = [P.sb([128, 4], F32, 'wgG', es=es) for _ in range(3)]
        ptr = P.ps([128, 8, 128], BF16, 'ptr', es=es)
        pq = [P.ps([128, 4, 128], F32, 'pq', es=es) for _ in range(2)]
        psc = [P.ps([128, 4, 128], F32, 'psc', es=es) for _ in range(2)]
        pacc = [P.ps([128, 512], F32, 'pacc', es=es) for _ in range(2)]

        scw_v = wk[:].rearrange("p (a b) -> p a b", b=128)
        candw_v = wk[:].rearrange("p (a b) -> p a b", b=256)
        cand_v = wk2[:].rearrange("p (a b) -> p a b", b=256)
        eq_v = wk2[:].rearrange("p (h a b) -> p h a b", a=16, b=16)

        def idle(n):
            for _ in range(n):
                yield

        def routing(t):
            x_ = xt[t % 2]; tf_ = tf[t % 2]; idx_ = idx[t % 2]; gt_ = gt[t % 2]; tb = tbs[t % 2]
            P.dma('sp', x_, x_[:], xin_b, xin_b[t * 128:(t + 1) * 128, :])
            yield from idle(10)
            P.op('dve', lambda e: e.scalar_tensor_tensor(out=tmp[:], in0=x_[:], scalar=1.0, in1=x_[:], op0=ALU.mult, op1=ALU.mult,
                                                         accum_out=ss[:]), [x_], [tmp, ss])
            P.op('dve', lambda e: e.memset(trail[:, 1:2], 0.0), [], [trail, ss]); yield
            yield
            P.op('act', lambda e: e.activation(out=ss[:], in_=ss[:], func=AF.Sqrt, scale=1.0 / 1024, bias=P.epsb[:]), [ss, P.epsb], [ss])
            yield from idle(3)
            P.op('dve', lambda e: e.reciprocal(out=rstd[:], in_=ss[:]), [ss], [rstd]); yield
            P.op('dve', lambda e: e.scalar_tensor_tensor(out=tmp[:], in0=x_[:], scalar=rstd[:, 0:1], in1=mods['A2'][:],
                                                         op0=ALU.mult, op1=ALU.mult), [x_, rstd, mods['A2']], [tmp]); yield
            P.op('dve', lambda e: e.tensor_tensor(out=tf_[:], in0=tmp[:], in1=mods['B2'][:], op=ALU.add), [tmp, mods['B2']], [tf_])
            yield from idle(2)
            P.op('act', lambda e: e.copy(out=tb[:], in_=tf_[:]), [tf_], [tb])
            yield from idle(2)
            transpose_tile(P, tb, ptr, identb, tT, tT[:])
            yield from idle(3)
            for g4 in range(4):
                p_ = pq[g4 % 2]
                for j in range(4):
                    hs = g4 * 4 + j
                    for kc in range(8):
                        P.op('pe', lambda e: e.matmul(p_[:, j, :], lhsT=wq[:, kc, hs * 128:(hs + 1) * 128], rhs=tT[:, kc, :],
                                                      start=(kc == 0), stop=(kc == 7)), [wq, tT], [p_])
                yield from idle(3)
                P.op('act', lambda e: e.copy(out=qTb[:, g4 * 4:(g4 + 1) * 4, :], in_=p_[:]), [p_], [qTb]); yield
            yield from idle(2)
            for g4 in range(4):
                p_ = psc[g4 % 2]
                for j in range(4):
                    hs = g4 * 4 + j
                    P.op('pe', lambda e: e.matmul(p_[:, j, :], lhsT=qTb[:, hs, :], rhs=kb[:, hs, :], start=True, stop=True), [qTb, kb], [p_])
                yield from idle(2)
                P.op('act', lambda e: e.copy(out=scs[:, g4 * 4:(g4 + 1) * 4, :], in_=p_[:]), [p_], [scs]); yield
            yield from idle(2)
            for hs in range(16):
                P.op('dve', lambda e: e.max(out=tv[:, hs, 0:8], in_=scs[:, hs, :]), [scs], [tv])
                P.op('dve', lambda e: e.max_index(out=ti[:, hs, 0:8], in_max=tv[:, hs, 0:8], in_values=scs[:, hs, :]), [scs, tv], [ti]); yield
                P.op('dve', lambda e: e.match_replace(out=scw_v[:, hs, :], in_to_replace=tv[:, hs, 0:8], in_values=scs[:, hs, :], imm_value=-1e30), [scs, tv], [wk])
                P.op('dve', lambda e: e.max(out=tv[:, hs, 8:16], in_=scw_v[:, hs, :]), [wk], [tv]); yield
                P.op('dve', lambda e: e.max_index(out=ti[:, hs, 8:16], in_max=tv[:, hs, 8:16], in_values=scw_v[:, hs, :]), [wk, tv], [ti]); yield
            P.op('dve', lambda e: e.tensor_copy(out=tif[:], in_=ti[:]), [ti], [tif])
            tvv = tv[:].rearrange("p (h s) k -> p h s k", s=2)
            tiv = tif[:].rearrange("p (h s) k -> p h s k", s=2)
            candv = wk2[:].rearrange("p (h a b) -> p h a b", a=16, b=16)
            P.op('dve', lambda e: e.tensor_tensor(out=candv, in0=tvv[:, :, 0, :].unsqueeze(3).to_broadcast([128, 8, 16, 16]),
                                                  in1=tvv[:, :, 1, :].unsqueeze(2).to_broadcast([128, 8, 16, 16]), op=ALU.add), [tv], [wk2]); yield
            for h in range(8):
                P.op('dve', lambda e: e.max(out=cv[:, h, 0:8], in_=cand_v[:, h, :]), [wk2], [cv])
                P.op('dve', lambda e: e.max_index(out=ci[:, h, 0:8], in_max=cv[:, h, 0:8], in_values=cand_v[:, h, :]), [wk2, cv], [ci]); yield
                P.op('dve', lambda e: e.match_replace(out=candw_v[:, h, :], in_to_replace=cv[:, h, 0:8], in_values=cand_v[:, h, :], imm_value=-1e30), [wk2, cv], [wk])
                P.op('dve', lambda e: e.max(out=cv[:, h, 8:16], in_=candw_v[:, h, :]), [wk], [cv]); yield
                P.op('dve', lambda e: e.max_index(out=ci[:, h, 8:16], in_max=cv[:, h, 8:16], in_values=candw_v[:, h, :]), [wk, cv], [ci]); yield
            P.op('dve', lambda e: e.tensor_scalar(out=au[:], in0=ci[:], scalar1=cu[:, 0:1], scalar2=None, op0=ALU.logical_shift_right), [ci, cu], [au])
            P.op('dve', lambda e: e.tensor_scalar(out=bu[:], in0=ci[:], scalar1=cu[:, 1:2], scalar2=None, op0=ALU.bitwise_and), [ci, cu], [bu]); yield
            P.op('dve', lambda e: e.tensor_copy(out=af[:], in_=au[:]), [au], [af])
            P.op('dve', lambda e: e.tensor_copy(out=bf[:], in_=bu[:]), [bu], [bf]); yield
            for s_, xf in ((0, af), (1, bf)):
                P.op('dve', lambda e: e.tensor_tensor(out=eq_v, in0=xf[:].unsqueeze(3).to_broadcast([128, 8, 16, 16]),
                                                      in1=io16[:].unsqueeze(1).unsqueeze(1).to_broadcast([128, 8, 16, 16]), op=ALU.is_equal), [xf, io16], [wk2]); yield
                P.op('dve', lambda e: e.tensor_tensor(out=eq_v, in0=eq_v, in1=tiv[:, :, s_, :].unsqueeze(2).to_broadcast([128, 8, 16, 16]), op=ALU.mult), [wk2, tif], [wk2]); yield
                P.op('dve', lambda e: e.tensor_reduce(out=sel[:, s_, :, :], in_=eq_v, axis=AX.X, op=ALU.add), [wk2], [sel]); yield
            P.op('dve', lambda e: e.scalar_tensor_tensor(out=idxf[:], in0=sel[:, 0, :, :].rearrange("p h k -> p (h k)"), scalar=128.0,
                                                         in1=sel[:, 1, :, :].rearrange("p h k -> p (h k)"), op0=ALU.mult, op1=ALU.add), [sel], [idxf])
            if l > 0:
                P.op('dve', lambda e: e.tensor_scalar(out=idxf[:], in0=idxf[:], scalar1=float(l * NEXP), scalar2=None, op0=ALU.add), [idxf], [idxf])
            P.op('dve', lambda e: e.tensor_copy(out=idx_[:], in_=idxf[:]), [idxf], [idx_]); yield
            P.op('dve', lambda e: e.tensor_tensor(out=gt_[:], in0=cv[:], in1=cv[:, :, 0:1].to_broadcast([128, 8, 16]), op=ALU.subtract), [cv], [gt_])
            P.op('act', lambda e: e.activation(out=gt_[:], in_=gt_[:], func=AF.Exp), [gt_], [gt_]); yield
            P.op('dve', lambda e: e.tensor_reduce(out=gs[:], in_=gt_[:], axis=AX.X, op=ALU.add), [gt_], [gs])
            P.op('dve', lambda e: e.reciprocal(out=gs[:], in_=gs[:]), [gs], [gs]); yield
            P.op('dve', lambda e: e.tensor_tensor(out=gt_[:], in0=gt_[:], in1=gs[:].unsqueeze(2).to_broadcast([128, 8, 16]), op=ALU.mult), [gt_, gs], [gt_]); yield

        def step(gen):
            if gen is not None:
                next(gen, None)

        def exhaust(gen):
            if gen is not None:
                for _ in gen:
                    pass

        exhaust(routing(0))
        ig = 0; iv = 0; ip = 0
        P.op('act', lambda e: e.copy(out=trailA[:], in_=trail[:]), [trail], [trailA])
        for t in range(NT):
            x_ = xt[t % 2]; tf_ = tf[t % 2]; idx_ = idx[t % 2]; gt_ = gt[t % 2]; acc_ = acc[t % 2]; tb_ = tbs[t % 2]
            nxt = routing(t + 1) if t + 1 < NT else None
            slots = {}
            for gi in range(33):
                if gi < 32:
                    dg_ = dotsG[gi % 3]
                    for s4 in range(4):
                        j = gi * 4 + s4
                        g_ = gb[ig % NB]; ig += 1
                        slots[j] = g_
                        P.dma('pool', g_, None, uv, None,
                              fn=lambda e: e.indirect_dma_start(out=g_[:], out_offset=None, in_=uv[:, :],
                                                                in_offset=bass.IndirectOffsetOnAxis(ap=idx_[:, j:j + 1], axis=0)), extra_reads=[idx_])
                        pr_ = prodb[ip % 4]; ip += 1
                        P.op('dve', lambda e: e.tensor_tensor(out=pr_[:], in0=g_[:, 0:1024], in1=tb_[:], op=ALU.mult), [g_, tb_], [pr_])
                        P.op('act', lambda e: e.activation(out=pjunk[:], in_=pr_[:], func=AF.Identity, accum_out=dg_[:, s4:s4 + 1]),
                             [pr_], ([dg_] if s4 == 0 else []))
                        step(nxt)
                        step(nxt)
                    P.op('act', lambda e: e.copy(out=trailA[:, 0:1], in_=trailA[:, 1:2]), [], [trailA, dg_])
                if gi >= 1:
                    gp = gi - 1
                    dp_ = dotsG[gp % 3]; w_ = wgG[gp % 3]
                    P.op('act', lambda e: e.activation(out=w_[:], in_=dp_[:], func=AF.Gelu_apprx_tanh), [dp_], [w_])
                    P.op('dve', lambda e: e.tensor_tensor(out=w_[:], in0=w_[:], in1=gt_[:].rearrange("p h k -> p (h k)")[:, gp * 4:gp * 4 + 4], op=ALU.mult), [w_, gt_], [w_])
                    for s4 in range(4):
                        j = gp * 4 + s4
                        g_ = slots.pop(j)
                        d_ = dg[iv % 4]; iv += 1
                        P.op('dve', lambda e: e.tensor_scalar(out=d_[:], in0=identb[:], scalar1=w_[:, s4:s4 + 1], scalar2=None, op0=ALU.mult), [identb, w_], [d_])
                        for hf in range(2):
                            P.op('pe', lambda e: e.matmul(pacc[hf][:], lhsT=d_[:], rhs=g_[:, 1024 + hf * 512:1024 + (hf + 1) * 512],
                                                          start=(j == 0), stop=(j == 127)), [d_, g_], [pacc[hf]])
            exhaust(nxt)
            for hf in range(2):
                sl = slice(hf * 512, (hf + 1) * 512)
                P.op('dve', lambda e: e.tensor_tensor(out=acc_[:, sl], in0=pacc[hf][:], in1=mods['G2'][:, sl], op=ALU.mult), [pacc[hf], mods['G2']], [acc_])
            P.op('dve', lambda e: e.tensor_tensor(out=acc_[:], in0=acc_[:], in1=x_[:], op=ALU.add), [acc_, x_], [acc_])
            if final:
                rms_rstd(P, acc_[:], 1024, ss2, rstd2, tmp, [acc_])
                P.op('dve', lambda e: e.scalar_tensor_tensor(out=acc_[:], in0=acc_[:], scalar=rstd2[:, 0:1], in1=fg[:],
                                                             op0=ALU.mult, op1=ALU.mult), [acc_, rstd2, fg], [acc_])
            P.store('sp', xout_b[t * 128:(t + 1) * 128, :], acc_, acc_[:])
        P.barrier()


def phase_l1_sgu(P, io, mods, xin_b, xout_b, uv=None):
    with ExitStack() as es:
        identb = make_ident(P, es, BF16)
        win = load_weight_bf16(P, es, io['odd_w_in'], io['odd_w_in'][:, :], 2048, 'oddwin_bf')
        wout = load_weight_bf16(P, es, io['odd_w_out'], io['odd_w_out'][:, :], 1024, 'oddwout_bf')
        wst = P.sb([128, 8, 128], F32, 'wst', es=es)
        P.dma('sp', wst, wst[:], io['wsT'], io['wsT'][:, :, :])
        wsb = P.sb([128, 8, 128], BF16, 'wsb', es=es)
        P.op('dve', lambda e: e.tensor_copy(out=wsb[:], in_=wst[:]), [wst], [wsb])
        bsT = P.sb([128, 8], F32, 'bsT', es=es)
        P.dma('sp', bsT, bsT[:], io['bsT'], io['bsT'][:, :])
        binb = P.sb([128, 2048], F32, 'binb', es=es)
        P.dma('sp', binb, binb[:], io['odd_b_in'], io['odd_b_in'][0:1, :].partition_broadcast(128))
        ngb = P.sb([128, 1024], F32, 'ngb', es=es)
        P.dma('sp', ngb, ngb[:], io['sgu_norm_g'], io['sgu_norm_g'][0:1, :].partition_broadcast(128))
        xt = [P.sb([128, 1024], F32, 'xt', es=es) for _ in range(2)]
        tmp = P.sb([128, 1024], F32, 'tmp', es=es)
        hbf = P.sb([128, 1024], BF16, 'hbf', es=es)
        ss = P.sb([128, 1], F32, 'ss', es=es); rstd = P.sb([128, 1], F32, 'rstd', es=es); junk = P.sb([128, 1024], F32, 'junk', es=es)
        hT = P.sb([128, 8, 128], BF16, 'hT', es=es)
        zf = P.sb([128, 2048], F32, 'zf', es=es)
        vn = P.sb([128, 1024], BF16, 'vn', es=es)
        gat = P.sb([128, 1024], BF16, 'gat', es=es)
        gT = P.sb([128, 8, 128], BF16, 'gT', es=es)
        yt = [P.sb([128, 1024], F32, 'yt', es=es) for _ in range(2)]
        ptr = P.ps([128, 8, 128], BF16, 'ptr', es=es)
        pz = [P.ps([128, 512], F32, 'pz', es=es) for _ in range(4)]
        psg = [P.ps([128, 4, 128], F32, 'psg', es=es) for _ in range(2)]
        cg = conv_gen(P, io, uv, es, layers=(1,), R=2) if uv is not None else None

        def stepn(n):
            if cg is not None:
                for _ in range(n):
                    next(cg, None)
        for t in range(NT):
            x_ = xt[t % 2]; y_ = yt[t % 2]
            stepn(4)
            P.dma('sp', x_, x_[:], xin_b, xin_b[t * 128:(t + 1) * 128, :])
            norm_mod_tile(P, x_, mods['A1'], mods['B1'], (ss, rstd, junk), tmp, hbf)
            transpose_tile(P, hbf, ptr, identb, hT, hT[:])
            for n4 in range(4):
                p_ = pz[n4]
                for kc in range(8):
                    P.op('pe', lambda e: e.matmul(p_[:], lhsT=hT[:, kc, :], rhs=win[:, kc, n4 * 512:(n4 + 1) * 512],
                                                  start=(kc == 0), stop=(kc == 7)), [hT, win], [p_])
                sl = slice(n4 * 512, (n4 + 1) * 512)
                P.op('dve', lambda e: e.tensor_tensor(out=zf[:, sl], in0=p_[:], in1=binb[:, sl], op=ALU.add), [p_, binb], [zf])
            stepn(4)
            P.op('act', lambda e: e.activation(out=zf[:], in_=zf[:], func=AF.Gelu_apprx_tanh), [zf], [zf])
            rms_rstd(P, zf[:, 1024:2048], 1024, ss, rstd, junk, [zf])
            P.op('dve', lambda e: e.scalar_tensor_tensor(out=vn[:], in0=zf[:, 1024:2048], scalar=rstd[:, 0:1], in1=ngb[:],
                                                         op0=ALU.mult, op1=ALU.mult), [zf, rstd, ngb], [vn])
            for g in range(8):
                p_ = psg[g // 4]
                P.op('pe', lambda e: e.matmul(p_[:, g % 4, :], lhsT=wsb[:, g, :], rhs=vn[:, g * 128:(g + 1) * 128], start=True, stop=True), [wsb, vn], [p_])
            for g in range(8):
                p_ = psg[g // 4]
                P.op('dve', lambda e: e.scalar_tensor_tensor(out=gat[:, g * 128:(g + 1) * 128], in0=p_[:, g % 4, :], scalar=bsT[:, g:g + 1],
                                                             in1=zf[:, g * 128:(g + 1) * 128], op0=ALU.add, op1=ALU.mult), [p_, bsT, zf], [gat])
            if t == 0 and getattr(P, 'dbg_sgu', None) is not None:
                dd_ = P.dbg_sgu
                P.dma('sp', dd_, dd_[0, :, :], zf, zf[:], nowaw=True)
                vf_ = P.sb([128, 2048], F32, 'dbgvf', es=es)
                P.op('dve', lambda e: e.tensor_copy(out=vf_[:, 0:1024], in_=vn[:]), [vn], [vf_])
                P.op('dve', lambda e: e.tensor_copy(out=vf_[:, 1024:2048], in_=gat[:]), [gat], [vf_])
                P.dma('sp', dd_, dd_[1, :, :], vf_, vf_[:], nowaw=True)
            stepn(4)
            transpose_tile(P, gat, ptr, identb, gT, gT[:])
            stepn(4)
            for hf in range(2):
                p_ = pz[hf]
                for kc in range(8):
                    P.op('pe', lambda e: e.matmul(p_[:], lhsT=gT[:, kc, :], rhs=wout[:, kc, hf * 512:(hf + 1) * 512],
                                                  start=(kc == 0), stop=(kc == 7)), [gT, wout], [p_])
                sl = slice(hf * 512, (hf + 1) * 512)
                P.op('dve', lambda e: e.tensor_tensor(out=y_[:, sl], in0=p_[:], in1=mods['G1'][:, sl], op=ALU.mult), [p_, mods['G1']], [y_])
            P.op('pool', lambda e: e.tensor_tensor(out=y_[:], in0=y_[:], in1=x_[:], op=ALU.add), [y_, x_], [y_])
            P.store('sp', xout_b[t * 128:(t + 1) * 128, :], y_, y_[:])
        if cg is not None:
            for _ in cg:
                pass
        P.barrier()


IN_SPECS = [
    ('x', [NPOS, D], F32), ('ctx', [NCTX, D], F32), ('cT', [128, 8], F32), ('ccT', [128, 8], F32),
    ('ada_w', [2, D, 6 * D], F32), ('ada_b', [2, 6 * D], F32), ('norm1_g', [2, D], F32), ('norm2_g', [2, D], F32),
    ('final_g', [1, D], F32), ('w_in', [D, 4096], F32), ('w_out', [D, D], F32), ('dlam', [1, 256], F32), ('head_g', [1, 128], F32),
    ('cosT', [128, NPOS], F32), ('sinT', [128, NPOS], F32), ('c4s4', [128, 2, 128], BF16), ('dft', [2, NPOS, OWN], BF16),
    ('odd_w_in', [D, 2048], F32), ('odd_b_in', [1, 2048], F32), ('sgu_norm_g', [1, D], F32), ('wsT', [128, 8, 128], F32),
    ('bsT', [128, 8], F32), ('odd_w_out', [D, D], F32),
    ('peer_wq', [2, D, 2048], F32), ('keysT', [2, 128, 16, 128], F32), ('peer_u', [2, NEXP, D], F32), ('peer_v', [2, NEXP, D], F32),
]
TEST_SMALL_PEER = False


def build(stop=None, debug_outs=()):
    nc = bass.Bass("TRN2", target_bir_lowering=False)
    with ExitStack() as es:
        P = Prog(nc, es)
        io = {}
        for name, shape, dt in IN_SPECS:
            if TEST_SMALL_PEER and name in ('peer_u', 'peer_v'):
                shape = [2, 128, D]
            io[name] = P.dram(name, shape, dt, kind="ExternalInput")
        out = P.dram('out', [OWN, D], F32, kind="ExternalOutput")
        def scr(name, shape, dt):
            return P.dram(name, shape, dt, kind=("ExternalOutput" if name in debug_outs else "Internal"))
        sc = {
            'kT': scr('kT_d', [6, 128, NKEY], BF16), 'v': scr('v_d', [NKEY, 768], BF16), 'qT': scr('qT_d', [6, 128, OWN], BF16),
            'fT': scr('fT_d', [2, 128, NPOS], BF16), 'mixT': scr('mixT_d', [8, 128, OWN], BF16),
            'uv': scr('uv_d', [2 * NEXP, 2 * D], BF16),
            'x1': scr('x1_d', [OWN, D], F32), 'x2': scr('x2_d', [OWN, D], F32), 'x3': scr('x3_d', [OWN, D], F32),
        }
        P.epsb = P.sb([128, 1], F32, 'epsb')
        P.op('dve', lambda e: e.memset(P.epsb[:], EPS), [], [P.epsb])
        mods = {n: P.sb([128, 1024], F32, 'mod_' + n) for n in ['A1', 'B1', 'G1', 'A2', 'B2', 'G2', 'A1c', 'B1c']}
        finals = [out]

        def run():
            phase_mod(P, io, 0, mods, True)
            if 'mods_d' in debug_outs:
                md = P.dram('mods_d', [8, 128, 1024], F32, kind="ExternalOutput")
                for i, n in enumerate(['A1', 'B1', 'G1', 'A2', 'B2', 'G2', 'A1c', 'B1c']):
                    P.dma('sp', md, md[i, :, :], mods[n], mods[n][:], nowaw=True)
                finals.append(md)
            if stop == 'mod0': return
            phase_l0_proj(P, io, mods, sc)
            if stop == 'proj': return
            phase_l0_attn(P, io, sc)
            if stop == 'attn': return
            phase_l0_fourier(P, io, sc)
            if stop == 'fourier': return
            phase_wout_resid(P, io, mods, io['w_out'], sc['mixT'], io['x'], io['x'][0:OWN, :], sc['x1'])
            if stop == 'x1': return
            phase_peer(P, io, 0, mods, sc['x1'], sc['x2'], False, sc['uv'])
            if stop == 'x2': return
            phase_mod(P, io, 1, mods, False)
            if 'sgu_d' in debug_outs:
                P.dbg_sgu = P.dram('sgu_d', [2, 128, 2048], F32, kind="ExternalOutput"); finals.append(P.dbg_sgu)
            phase_l1_sgu(P, io, mods, sc['x2'], sc['x3'], sc['uv'])
            if stop == 'x3': return
            phase_peer(P, io, 1, mods, sc['x3'], out, True, sc['uv'])
        run()
        P.finish(list(sc.values()) + finals)
    return nc


def host_consts():
    pos = np.arange(NPOS)
    r = (pos // 64).astype(np.float32); col = (pos % 64).astype(np.float32)
    inv = (np.float32(10000.0) ** (-np.arange(16, dtype=np.float32) / np.float32(16))).astype(np.float32)
    ar = r[:, None] * inv; ac = col[:, None] * inv
    ang = np.concatenate([ar, ar, ac, ac], axis=-1).astype(np.float32)
    cos = np.cos(ang).astype(np.float32); sin = np.sin(ang).astype(np.float32)
    sign = np.concatenate([-np.ones(16), np.ones(16), -np.ones(16), np.ones(16)]).astype(np.float32)
    cosT = np.ascontiguousarray(np.concatenate([cos.T, cos.T], axis=0))
    sinT = np.ascontiguousarray(np.concatenate([(sin * sign).T, (sin * sign).T], axis=0))
    src = np.concatenate([np.arange(16, 32), np.arange(0, 16), np.arange(48, 64), np.arange(32, 48)])
    jj = np.arange(64)
    a4 = 2.0 * np.pi * np.outer(jj, jj) / 64.0
    c4 = np.zeros((128, 2, 128), np.float64)
    for b in range(2):
        c4[b * 64:(b + 1) * 64, 0, b * 64:(b + 1) * 64] = np.cos(a4)
        c4[b * 64:(b + 1) * 64, 1, b * 64:(b + 1) * 64] = -np.sin(a4)
    c4 = c4.astype(ml_dtypes.bfloat16)
    dfts = []
    for half in range(2):
        n = (np.arange(NPOS, dtype=np.int64) + half * OWN) % NPOS
        k = np.arange(half * OWN, (half + 1) * OWN, dtype=np.int64)
        ph = (np.outer(n, k) % NPOS).astype(np.float64) * (2.0 * np.pi / NPOS)
        dfts.append(np.stack([np.cos(ph), np.sin(ph)]).astype(ml_dtypes.bfloat16))
    return cosT, sinT, src, c4, dfts


def make_in_maps(inp, cores):
    cosT, sinT, src, c4, dfts = host_consts()
    w_in = inp['even_w_in'][0]
    permcols = np.concatenate([h * 64 + src for h in range(12)])
    w_ext = np.ascontiguousarray(np.concatenate([w_in, w_in[:, 0:768][:, permcols], w_in[:, 768:1536][:, permcols]], axis=1))
    keysT = np.ascontiguousarray(inp['peer_keys'].reshape(2, 16, 128, 128).transpose(0, 3, 1, 2))
    shared = {
        'ccT': np.ascontiguousarray(inp['c_ctx'].reshape(8, 128).T),
        'ada_w': inp['ada_w'], 'ada_b': inp['ada_b'], 'norm1_g': inp['norm1_g'], 'norm2_g': inp['norm2_g'],
        'final_g': inp['final_g'].reshape(1, D), 'w_in': w_ext, 'w_out': inp['even_w_out'][0],
        'dlam': inp['diff_lambda'].reshape(1, 256), 'head_g': inp['diff_norm_g'].reshape(1, 128),
        'cosT': cosT, 'sinT': sinT, 'c4s4': c4,
        'odd_w_in': inp['odd_w_in'][0], 'odd_b_in': inp['odd_b_in'].reshape(1, 2048), 'sgu_norm_g': inp['sgu_norm_g'].reshape(1, D),
        'wsT': np.ascontiguousarray(inp['sgu_w'][0].transpose(2, 0, 1)), 'bsT': np.ascontiguousarray(inp['sgu_b'][0].T),
        'odd_w_out': inp['odd_w_out'][0], 'peer_wq': inp['peer_wq'], 'keysT': keysT, 'peer_u': inp['peer_u'], 'peer_v': inp['peer_v'],
    }
    maps = []
    for core in cores:
        b, half = core // 2, core % 2
        m = dict(shared)
        order = (np.arange(NPOS) + half * OWN) % NPOS
        m['x'] = np.ascontiguousarray(inp['x'][b][order]); m['cosT'] = np.ascontiguousarray(cosT[:, order])
        m['sinT'] = np.ascontiguousarray(sinT[:, order]); m['ctx'] = np.ascontiguousarray(inp['ctx'][b])
        m['cT'] = np.ascontiguousarray(inp['c'][b].reshape(8, 128).T)
        m['dft'] = dfts[half]
        maps.append(m)
    return maps


def kernel(**inputs):
    inp = {k: np.asarray(v) for k, v in inputs.items()}
    nc = build()
    maps = make_in_maps(inp, list(range(8)))
    res = run_bass_kernel_spmd(nc, maps, core_ids=list(range(8)))
    out = np.zeros((4, NPOS, D), np.float32)
    for core in range(8):
        b, half = core // 2, core % 2
        out[b, half * OWN:(half + 1) * OWN, :] = res.results[core]['out']
    return out
```

```python
import math
from contextlib import ExitStack
import numpy as np
import ml_dtypes
import concourse.bass as bass
import concourse.mybir as mybir
from concourse.bass_utils import run_bass_kernel_spmd

F32 = mybir.dt.float32; BF16 = mybir.dt.bfloat16; U32 = mybir.dt.uint32; I32 = mybir.dt.int32
AF = mybir.ActivationFunctionType; ALU = mybir.AluOpType; AX = mybir.AxisListType

D = 1024; NPOS = 4096; NCTX = 256; NKEY = NPOS + NCTX; OWN = 2048; NT = OWN // 128
EPS = 1e-6
NEXP = 16384


class Buf:
    __slots__ = ('ap', 'name', 'lastw', 'readers', 'sem', 'cnt', 'uid', 'ssem', 'scnt')
    _serial = [0]

    def __init__(self, ap, name):
        self.ap = ap; self.name = name; self.lastw = None; self.readers = {}; self.sem = None; self.cnt = 0; self.ssem = None; self.scnt = 0
        Buf._serial[0] += 1; self.uid = Buf._serial[0]

    def __getitem__(self, k):
        return self.ap[k]


class Prog:
    def __init__(self, nc, es):
        self.nc = nc; self.es = es
        self.eng = {'pe': nc.tensor, 'act': nc.scalar, 'dve': nc.vector, 'pool': nc.gpsimd, 'sp': nc.sync}
        self.esem = {e: es.enter_context(nc.semaphore('sem_' + e)) for e in self.eng}
        self.ecnt = {e: 0 for e in self.eng}
        self.waited = {}
        self.nsem = 0
        self.dmabufs = []
        self.storebufs = {}
        self.free_sems = []
        self.nb = 0

    def sb(self, shape, dt, name=None, es=None):
        self.nb += 1
        name = name or ('b%d' % self.nb)
        t = (es or self.es).enter_context(self.nc.sbuf_tensor(name + '_%d' % self.nb, list(shape), dt))
        return Buf(t, name)

    def ps(self, shape, dt, name=None, es=None):
        self.nb += 1
        name = name or ('p%d' % self.nb)
        t = (es or self.es).enter_context(self.nc.psum_tensor(name + '_%d' % self.nb, list(shape), dt))
        return Buf(t, name)

    def dram(self, name, shape, dt, kind="Internal"):
        t = self.nc.dram_tensor(name, list(shape), dt, kind=kind)
        return Buf(t.ap(), name)

    def _wait(self, e, ev):
        key = (e, ev[2])
        if self.waited.get(key, 0) >= ev[1]:
            return
        self.eng[e].wait_ge(ev[0], ev[1]); self.waited[key] = ev[1]

    def _deps(self, e, reads, writes):
        evs = []
        for b in reads:
            if b.lastw is not None: evs.append(b.lastw)
        for b in writes:
            if b.lastw is not None: evs.append(b.lastw)
            evs.extend(b.readers.values())
        for ev in evs:
            if e == 'pe' and ev[2] == 'pe': continue
            self._wait(e, ev)

    def op(self, e, fn, reads=(), writes=()):
        self._deps(e, reads, writes)
        ins = fn(self.eng[e])
        self.ecnt[e] += 1
        ins.then_inc(self.esem[e], 1)
        ev = (self.esem[e], self.ecnt[e], e)
        for b in writes: b.lastw = ev; b.readers = {}
        for b in reads:
            if b not in writes: b.readers[e] = ev
        return ins

    def _slot(self):
        if self.free_sems:
            return self.free_sems.pop()
        self.nsem += 1
        return [self.es.enter_context(self.nc.semaphore('dsem%d' % self.nsem)), 0, 'dsem%d' % self.nsem]

    def dma(self, q, out_b, out_ap, in_b, in_ap, fn=None, extra_reads=(), nowaw=False):
        if out_b.sem is None:
            out_b.sem = self._slot()
        slot = out_b.sem
        key = slot[2]
        saved = None
        if nowaw and out_b.lastw is not None and out_b.lastw[2] == key:
            saved = out_b.lastw; out_b.lastw = None
        self._deps(q, [in_b] + list(extra_reads), [out_b])
        if saved is not None:
            out_b.lastw = saved
        slot[1] += 1
        if fn is None:
            ins = self.eng[q].dma_start(out=out_ap, in_=in_ap)
        else:
            ins = fn(self.eng[q])
        ins.then_inc(slot[0], 16)
        ev = (slot[0], 16 * slot[1], key)
        out_b.lastw = ev
        if not nowaw: out_b.readers = {}
        in_b.readers[key] = ev
        for b in extra_reads: b.readers[key] = ev
        self.dmabufs.append(out_b)

    def store(self, q, out_ap, in_b, in_ap):
        self._deps(q, [in_b], [])
        if in_b.ssem is None:
            in_b.ssem = self._slot()
        slot = in_b.ssem
        slot[1] += 1
        self.eng[q].dma_start(out=out_ap, in_=in_ap).then_inc(slot[0], 16)
        in_b.readers[slot[2]] = (slot[0], 16 * slot[1], slot[2])
        self.storebufs[in_b.uid] = in_b

    def finish(self, bufs):
        for b in bufs:
            if b.lastw is not None: self._wait('sp', b.lastw)

    def barrier(self):
        evs = [(self.esem[e], self.ecnt[e], e) for e in self.eng if self.ecnt[e] > 0]
        slots = {}
        for b in self.dmabufs:
            if b.sem is not None:
                slots[b.sem[2]] = b.sem; b.sem = None
        for b in self.storebufs.values():
            if b.ssem is not None:
                slots[b.ssem[2]] = b.ssem; b.ssem = None
        self.dmabufs = []; self.storebufs = {}
        for sl in slots.values():
            if sl[1] > 0:
                evs.append((sl[0], 16 * sl[1], sl[2]))
        for e in self.eng:
            for ev in evs:
                self._wait(e, ev)
        self.free_sems.extend(slots.values())


def make_ident(P, es, dt):
    idf = P.sb([128, 128], F32, 'identf', es=es)
    P.op('pool', lambda e: e.memset(idf[:], 0.0), [], [idf])
    P.op('pool', lambda e: e.affine_select(out=idf[:], in_=idf[:], pattern=[[-1, 128]], compare_op=ALU.not_equal,
                                           fill=1.0, base=0, channel_multiplier=1), [idf], [idf])
    if dt == F32:
        return idf
    idb = P.sb([128, 128], dt, 'identb', es=es)
    P.op('dve', lambda e: e.tensor_copy(out=idb[:], in_=idf[:]), [idf], [idb])
    return idb


def rms_rstd(P, src, ncol, ss, rstd, junk, srcb=()):
    P.op('act', lambda e: e.activation(out=junk[:, 0:ncol], in_=src, func=AF.Square), list(srcb), [junk])
    P.op('dve', lambda e: e.reduce_sum(out=ss[:], in_=junk[:, 0:ncol], axis=AX.X), [junk], [ss])
    P.op('act', lambda e: e.activation(out=ss[:], in_=ss[:], func=AF.Sqrt, scale=1.0 / ncol, bias=P.epsb[:]), [ss, P.epsb], [ss])
    P.op('dve', lambda e: e.reciprocal(out=rstd[:], in_=ss[:]), [ss], [rstd])


def phase_mod(P, io, l, mods, with_ctx):
    with ExitStack() as es:
        n_src = 2 if with_ctx else 1
        cT = P.sb([128, 2, 8], F32, 'cT', es=es)
        P.dma('sp', cT, cT[:, 0, :], io['cT'], io['cT'][:, :])
        lhs = []
        cs = P.sb([128, 2, 8], F32, 'cs', es=es)
        if with_ctx:
            cc = P.sb([128, 8], F32, 'cc', es=es)
            P.dma('sp', cc, cc[:], io['ccT'], io['ccT'][:, :])
            P.op('act', lambda e: e.activation(out=cs[:, 1, :], in_=cc[:], func=AF.Silu), [cc], [cs])
        P.op('act', lambda e: e.activation(out=cs[:, 0, :], in_=cT[:, 0, :], func=AF.Silu), [cT], [cs])
        for s in range(n_src):
            lb = P.sb([128, 8, 128], F32, 'lhsb', es=es)
            P.op('dve', lambda e: e.tensor_copy(out=lb[:], in_=cs[:, s, :].unsqueeze(2).to_broadcast([128, 8, 128])), [cs], [lb])
            lhs.append(lb)
        ones1 = P.sb([1, 128], F32, 'ones1', es=es)
        P.op('dve', lambda e: e.memset(ones1[:], 1.0), [], [ones1])
        bias = P.sb([1, 6144], F32, 'adab', es=es)
        P.dma('sp', bias, bias[:], io['ada_b'], io['ada_b'][l:l + 1, :])
        gb = [P.sb([128, 1024], F32, 'gbc', es=es) for _ in range(2)]
        P.dma('sp', gb[0], gb[0][:], io['norm1_g'], io['norm1_g'][l:l + 1, :].partition_broadcast(128))
        P.dma('sp', gb[1], gb[1][:], io['norm2_g'], io['norm2_g'][l:l + 1, :].partition_broadcast(128))
        wb = [P.sb([128, 8, 1024], F32, 'adaw', es=es) for _ in range(2)]
        pm = [P.ps([128, 512], F32, 'pm', es=es) for _ in range(4)]
        names = ['B1', 'A1', 'G1', 'B2', 'A2', 'G2']
        ip = 0
        for j in range(6):
            w = wb[j % 2]
            P.dma('sp', w, w[:], io['ada_w'], io['ada_w'][l, :, j * 1024:(j + 1) * 1024].rearrange("(kc p) n -> p kc n", p=128))
            for s in range(n_src):
                if s == 1 and j > 1:
                    continue
                dst = mods[names[j] + ('c' if s == 1 else '')]
                for half in range(2):
                    p_ = pm[ip % 4]; ip += 1
                    for kc in range(8):
                        P.op('pe', lambda e: e.matmul(p_[:], lhsT=lhs[s][:, kc, :], rhs=w[:, kc, half * 512:(half + 1) * 512],
                                                      start=(kc == 0), stop=False), [lhs[s], w], [p_])
                    P.op('pe', lambda e: e.matmul(p_[:], lhsT=ones1[0:1, :], rhs=bias[0:1, j * 1024 + half * 512: j * 1024 + (half + 1) * 512],
                                                  start=False, stop=True), [ones1, bias], [p_])
                    sl = slice(half * 512, (half + 1) * 512)
                    if names[j][0] == 'A':
                        g_ = gb[0] if j == 1 else gb[1]
                        P.op('dve', lambda e: e.scalar_tensor_tensor(out=dst[:, sl], in0=p_[:], scalar=1.0, in1=g_[:, sl],
                                                                     op0=ALU.add, op1=ALU.mult), [p_, g_], [dst])
                    else:
                        P.op('act', lambda e: e.copy(out=dst[:, sl], in_=p_[:]), [p_], [dst])
        P.barrier()


def norm_mod_tile(P, xt, A, B, rstd_bufs, tmp, hbf):
    ss, rstd, junk = rstd_bufs
    rms_rstd(P, xt[:], 1024, ss, rstd, junk, [xt])
    P.op('dve', lambda e: e.scalar_tensor_tensor(out=tmp[:], in0=xt[:], scalar=rstd[:, 0:1], in1=A[:],
                                                 op0=ALU.mult, op1=ALU.mult), [xt, rstd, A], [tmp])
    P.op('pool', lambda e: e.tensor_tensor(out=hbf[:], in0=tmp[:], in1=B[:], op=ALU.add), [tmp, B], [hbf])


def transpose_tile(P, src_bf, ptr, identb, dst, dst_ap):
    for kc in range(8):
        P.op('pe', lambda e: e.transpose(out=ptr[:, kc, :], in_=src_bf[:, kc * 128:(kc + 1) * 128], identity=identb[:]),
             [src_bf, identb], [ptr])
    P.op('act', lambda e: e.copy(out=dst_ap, in_=ptr[:]), [ptr], [dst])


def load_weight_bf16(P, es_phase, dram_b, dram_ap2d, ncols, name):
    wbf = P.sb([128, 8, ncols], BF16, name, es=es_phase)
    with ExitStack() as es:
        st = [P.sb([128, 8, 512], F32, 'wstage', es=es) for _ in range(2)]
        for i, c0 in enumerate(range(0, ncols, 512)):
            s_ = st[i % 2]
            P.dma('sp', s_, s_[:], dram_b, dram_ap2d[:, c0:c0 + 512].rearrange("(kc p) n -> p kc n", p=128))
            eng = 'dve' if i % 2 == 0 else 'pool'
            P.op(eng, lambda e: e.tensor_copy(out=wbf[:, :, c0:c0 + 512], in_=s_[:]), [s_], [wbf])
        P.barrier()
    return wbf


def phase_l0_proj(P, io, mods, sc):
    with ExitStack() as es:
        identb = make_ident(P, es, BF16)
        wbf = load_weight_bf16(P, es, io['w_in'], io['w_in'][:, :], 4096, 'w_in_bf')
        xt = [P.sb([128, 1024], F32, 'xt', es=es) for _ in range(2)]
        tmp = P.sb([128, 1024], F32, 'tmp', es=es)
        hbf = [P.sb([128, 1024], BF16, 'hbf', es=es) for _ in range(2)]
        ss = P.sb([128, 1], F32, 'ss', es=es); rstd = P.sb([128, 1], F32, 'rstd', es=es); junk = P.sb([128, 1024], F32, 'junk', es=es)
        hT = [P.sb([128, 8, 512], BF16, 'hT', es=es) for _ in range(2)]
        cosb = [P.sb([128, 512], F32, 'cos', es=es) for _ in range(2)]
        sinb = [P.sb([128, 512], F32, 'sin', es=es) for _ in range(2)]
        r1 = [P.sb([128, 512], F32, 'r1', es=es) for _ in range(2)]
        r2 = [P.sb([128, 512], F32, 'r2', es=es) for _ in range(2)]
        ko = [P.sb([128, 512], BF16, 'ko', es=es) for _ in range(3)]
        vo = [P.sb([128, 768], BF16, 'vo', es=es) for _ in range(2)]
        ptr = P.ps([128, 8, 128], BF16, 'ptr', es=es)
        pp = [P.ps([128, 512], F32, 'pp', es=es) for _ in range(4)]
        pv = [P.ps([128, 384], F32, 'pv', es=es) for _ in range(2)]
        cnt = {'pp': 0, 'ko': 0, 'x': 0, 'vo': 0}

        def proj_T(hT_, n, col0):
            p_ = pp[cnt['pp'] % 4]; cnt['pp'] += 1
            for kc in range(8):
                P.op('pe', lambda e: e.matmul(p_[:, 0:n], lhsT=wbf[:, kc, col0:col0 + 128], rhs=hT_[:, kc, 0:n],
                                              start=(kc == 0), stop=(kc == 7)), [wbf, hT_], [p_])
            return p_

        for g in range(9):
            n = 256 if g == 0 else 512
            ntile = n // 128
            T0 = 0 if g == 0 else NCTX + (g - 1) * 512
            pos0 = (g - 1) * 512
            hT_ = hT[g % 2]
            A = mods['A1c'] if g == 0 else mods['A1']
            B = mods['B1c'] if g == 0 else mods['B1']
            for t in range(ntile):
                x_ = xt[cnt['x'] % 2]; hb_ = hbf[cnt['x'] % 2]; cnt['x'] += 1
                if g == 0:
                    P.dma('sp', x_, x_[:], io['ctx'], io['ctx'][t * 128:(t + 1) * 128, :])
                else:
                    P.dma('sp', x_, x_[:], io['x'], io['x'][pos0 + t * 128: pos0 + (t + 1) * 128, :])
                norm_mod_tile(P, x_, A, B, (ss, rstd, junk), tmp, hb_)
                transpose_tile(P, hb_, ptr, identb, hT_, hT_[:, :, t * 128:(t + 1) * 128])
            own = g > 0 and pos0 < OWN
            if g > 0:
                cb = cosb[g % 2]; sb_ = sinb[g % 2]
                P.dma('sp', cb, cb[:], io['cosT'], io['cosT'][:, pos0:pos0 + 512])
                P.dma('sp', sb_, sb_[:], io['sinT'], io['sinT'][:, pos0:pos0 + 512])
            for which in (['k', 'q'] if own else ['k']):
                cbase = 768 if which == 'k' else 0
                pbase = 3328 if which == 'k' else 2560
                for c in range(6):
                    p1 = proj_T(hT_, n, cbase + c * 128)
                    o_ = ko[cnt['ko'] % 3]; cnt['ko'] += 1
                    if g == 0:
                        P.op('act', lambda e: e.copy(out=o_[:, 0:n], in_=p1[:, 0:n]), [p1], [o_])
                    else:
                        p2 = proj_T(hT_, n, pbase + c * 128)
                        a_ = r1[c % 2]; b_ = r2[c % 2]
                        P.op('dve', lambda e: e.tensor_tensor(out=a_[:], in0=p1[:], in1=cb[:], op=ALU.mult), [p1, cb], [a_])
                        P.op('dve', lambda e: e.tensor_tensor(out=b_[:], in0=p2[:], in1=sb_[:], op=ALU.mult), [p2, sb_], [b_])
                        P.op('pool', lambda e: e.tensor_tensor(out=o_[:], in0=a_[:], in1=b_[:], op=ALU.add), [a_, b_], [o_])
                    if which == 'k':
                        P.store('sp', sc['kT'][c, :, T0:T0 + n], o_, o_[:, 0:n])
                    else:
                        q0 = pos0
                        P.store('sp', sc['qT'][c, :, q0:q0 + 512], o_, o_[:, :])
            if g > 0:
                for c in range(2):
                    p1 = proj_T(hT_, n, 2304 + c * 128)
                    o_ = ko[cnt['ko'] % 3]; cnt['ko'] += 1
                    P.op('act', lambda e: e.copy(out=o_[:], in_=p1[:]), [p1], [o_])
                    P.store('sp', sc['fT'][c, :, pos0:pos0 + 512], o_, o_[:, :])
            for t in range(ntile):
                for hh in range(2):
                    for kc in range(8):
                        P.op('pe', lambda e: e.matmul(pv[hh][:], lhsT=hT_[:, kc, t * 128:(t + 1) * 128],
                                                      rhs=wbf[:, kc, 1536 + hh * 384: 1536 + (hh + 1) * 384],
                                                      start=(kc == 0), stop=(kc == 7)), [hT_, wbf], [pv[hh]])
                v_ = vo[cnt['vo'] % 2]; cnt['vo'] += 1
                P.op('act', lambda e: e.copy(out=v_[:, 0:384], in_=pv[0][:]), [pv[0]], [v_])
                P.op('dve', lambda e: e.tensor_copy(out=v_[:, 384:768], in_=pv[1][:]), [pv[1]], [v_])
                P.store('sp', sc['v'][T0 + t * 128: T0 + (t + 1) * 128, :], v_, v_[:])
        P.barrier()


def phase_l0_attn(P, io, sc, with_conv=True):
    lam_init = 0.8 - 0.6 * math.exp(-0.3 * 0.0)
    with ExitStack() as es:
        identb = make_ident(P, es, BF16)
        dl = P.sb([128, 256], F32, 'dl', es=es)
        P.dma('sp', dl, dl[:], io['dlam'], io['dlam'][0:1, :].partition_broadcast(128))
        lj = P.sb([128, 64], F32, 'lj', es=es)
        l2 = P.sb([128, 2], F32, 'l2', es=es)
        for i in range(2):
            P.op('dve', lambda e: e.tensor_tensor(out=lj[:], in0=dl[:, i * 128:i * 128 + 64], in1=dl[:, i * 128 + 64:i * 128 + 128], op=ALU.mult), [dl], [lj])
            P.op('dve', lambda e: e.reduce_sum(out=l2[:, i:i + 1], in_=lj[:], axis=AX.X), [lj], [l2])
        P.op('act', lambda e: e.activation(out=l2[:], in_=l2[:], func=AF.Exp), [l2], [l2])
        neglam = P.sb([128, 1], F32, 'neglam', es=es)
        P.op('dve', lambda e: e.tensor_tensor(out=neglam[:], in0=l2[:, 1:2], in1=l2[:, 0:1], op=ALU.subtract), [l2], [neglam])
        P.op('dve', lambda e: e.tensor_scalar(out=neglam[:], in0=neglam[:], scalar1=-lam_init, scalar2=None, op0=ALU.add), [neglam], [neglam])
        hg = P.sb([128, 128], F32, 'hg', es=es)
        P.dma('sp', hg, hg[:], io['head_g'], io['head_g'][0:1, :].partition_broadcast(128))
        P.op('dve', lambda e: e.tensor_scalar(out=hg[:], in0=hg[:], scalar1=1.0 - lam_init, scalar2=None, op0=ALU.mult), [hg], [hg])
        zer = P.sb([128, 512], BF16, 'zer', es=es)
        P.op('dve', lambda e: e.memset(zer[:], 0.0), [], [zer])

        kT = [P.sb([128, NKEY], BF16, 'kTc', es=es) for _ in range(2)]
        vx = [P.sb([128, 34, 130], BF16, 'vx', es=es) for _ in range(2)]
        qT = [P.sb([128, OWN], BF16, 'qTc', es=es) for _ in range(2)]
        mixc = [P.sb([128, OWN], BF16, 'mixc', es=es) for _ in range(2)]
        for v_ in vx:
            P.op('pool', lambda e: e.memset(v_[:, :, 128:130], 1.0), [], [v_])
        eT = [P.sb([128, 512], BF16, 'eT', es=es) for _ in range(3)]
        acc = [[P.ps([128, 512], F32, 'acc', es=es) for _ in range(2)] for _ in range(2)]
        sps = [P.ps([128, 512], F32, 'sps', es=es) for _ in range(3)]
        ptr = P.ps([128, 4, 128], BF16, 'ptr', es=es)
        sm = [P.sb([128, 8], F32, 'sm', es=es) for _ in range(2)]
        t1 = [P.sb([128, 128], F32, 't1', es=es) for _ in range(2)]
        dd = [P.sb([128, 128], F32, 'dd', es=es) for _ in range(2)]
        junk = P.sb([128, 128], F32, 'junk', es=es)
        obf = [P.sb([128, 128], BF16, 'obf', es=es) for _ in range(2)]
        accS = [P.sb([128, 4, 258], F32, 'accS', es=es) for _ in range(2)]
        neghalf = P.sb([128, 1], F32, 'neghalf', es=es)
        P.op('dve', lambda e: e.memset(neghalf[:], -0.5), [], [neghalf])
        trl = P.sb([128, 8], F32, 'trl', es=es)
        ie = 0
        cg = conv_gen(P, io, sc['uv'], es, layers=(0,)) if with_conv else None

        def load_head(c):
            P.dma('sp', kT[c % 2], kT[c % 2][:], sc['kT'], sc['kT'][c, :, :])
            P.dma('sp', vx[c % 2], vx[c % 2][:, :, 0:128], sc['v'], sc['v'][:, c * 128:(c + 1) * 128].rearrange("(kc p) e -> p kc e", p=128))
            P.dma('sp', qT[c % 2], qT[c % 2][:], sc['qT'], sc['qT'][c, :, :])

        sm4 = [P.sb([128, 8], F32, 'sm4', es=es) for _ in range(4)]
        t14 = [P.sb([128, 128], F32, 't14', es=es) for _ in range(4)]
        dd4 = [P.sb([128, 128], F32, 'dd4', es=es) for _ in range(4)]
        ob4 = [P.sb([128, 128], BF16, 'ob4', es=es) for _ in range(4)]

        def idle(n):
            for _ in range(n):
                yield

        def post(c, qg, aS, mx):
            yield from idle(3)
            for qt in range(4):
                o0 = (qt % 2) * 129; i0 = qt // 2; i1 = 2 + qt // 2
                s = sm4[qt]; t_ = t14[qt]; d_ = dd4[qt]
                P.op('dve', lambda e: e.reciprocal(out=s[:, 0:1], in_=aS[:, i0, o0 + 128:o0 + 129]), [aS], [s])
                P.op('dve', lambda e: e.reciprocal(out=s[:, 1:2], in_=aS[:, i1, o0 + 128:o0 + 129]), [aS], [s]); yield
                P.op('dve', lambda e: e.tensor_tensor(out=s[:, 2:3], in0=s[:, 1:2], in1=neglam[:], op=ALU.mult), [s, neglam], [s])
                P.op('dve', lambda e: e.tensor_scalar(out=t_[:], in0=aS[:, i1, o0:o0 + 128], scalar1=s[:, 2:3], scalar2=None, op0=ALU.mult), [aS, s], [t_]); yield
                P.op('dve', lambda e: e.scalar_tensor_tensor(out=d_[:], in0=aS[:, i0, o0:o0 + 128], scalar=s[:, 0:1], in1=t_[:],
                                                             op0=ALU.mult, op1=ALU.add), [aS, s, t_], [d_]); yield
                P.op('dve', lambda e: e.scalar_tensor_tensor(out=junk[:], in0=d_[:], scalar=1.0, in1=d_[:], op0=ALU.mult, op1=ALU.mult,
                                                             accum_out=s[:, 3:4]), [d_], [junk, s])
                P.op('dve', lambda e: e.memset(trl[:, 0:1], 0.0), [], [trl, s]); yield
            yield from idle(5)
            for qt in range(4):
                s = sm4[qt]
                P.op('act', lambda e: e.activation(out=s[:, 4:5], in_=s[:, 3:4], func=AF.Ln, scale=1.0 / 128, bias=P.epsb[:]), [s, P.epsb], [s])
                P.op('act', lambda e: e.activation(out=s[:, 5:6], in_=s[:, 4:5], func=AF.Exp, scale=-0.5), [s], [s]); yield
            yield from idle(5)
            for qt in range(4):
                s = sm4[qt]; d_ = dd4[qt]; ob = ob4[qt]
                P.op('dve', lambda e: e.scalar_tensor_tensor(out=ob[:], in0=d_[:], scalar=s[:, 5:6], in1=hg[:],
                                                             op0=ALU.mult, op1=ALU.mult), [d_, s, hg], [ob]); yield
            yield from idle(5)
            for qt in range(4):
                P.op('pe', lambda e: e.transpose(out=ptr[:, qt, :], in_=ob4[qt][:], identity=identb[:]), [ob4[qt], identb], [ptr])
            yield from idle(6)
            P.op('act', lambda e: e.copy(out=mx[:, qg * 512:(qg + 1) * 512], in_=ptr[:].rearrange("p a b -> p (a b)")), [ptr], [mx]); yield
            if qg == 3:
                P.store('sp', sc['mixT'][c, :, :], mx, mx[:]); yield

        def exhaust(gen):
            if gen is not None:
                for _ in gen:
                    pass

        pg = None; grp = 0
        load_head(0)
        for c in range(6):
            k_ = kT[c % 2]; v_ = vx[c % 2]; q_ = qT[c % 2]; mx = mixc[c % 2]
            if c + 1 < 6:
                load_head(c + 1)
            for qg in range(4):
                for m in range(2):
                    for hf in range(2):
                        P.op('pe', lambda e: e.matmul(acc[m][hf][:], lhsT=zer[:, 0:128], rhs=zer[:, :], start=True, stop=False,
                                                      skip_group_check=True), [zer], [acc[m][hf]])
                def pv(kc, m, e_):
                    for qt in range(4):
                        a_ = acc[m][qt // 2]
                        P.op('pe', lambda e: e.matmul(a_[:, (qt % 2) * 129:(qt % 2) * 129 + 129], lhsT=e_[:, qt * 128:(qt + 1) * 128],
                                                      rhs=v_[:, kc, 0:129], start=False, stop=(kc == 33), skip_group_check=True), [e_, v_], [a_])
                pend = None
                for kc in range(34):
                    for m in range(2):
                        s_ = sps[ie % 3]; e_ = eT[ie % 3]; ie += 1
                        P.op('pe', lambda e: e.matmul(s_[:], lhsT=k_[m * 64:(m + 1) * 64, kc * 128:(kc + 1) * 128],
                                                      rhs=q_[m * 64:(m + 1) * 64, qg * 512:(qg + 1) * 512], start=True, stop=True), [k_, q_], [s_])
                        P.op('act', lambda e: e.activation(out=e_[:], in_=s_[:], func=AF.Exp, scale=0.125), [s_], [e_])
                        if pend is not None:
                            pv(*pend)
                        pend = (kc, m, e_)
                        if cg is not None and ie % 3 == 0:
                            next(cg, None)
                        if pg is not None:
                            next(pg, None)
                pv(*pend)
                exhaust(pg)
                aS = accS[grp % 2]; grp += 1
                for m in range(2):
                    for hf in range(2):
                        if hf == 0:
                            P.op('act', lambda e: e.copy(out=aS[:, m * 2 + hf, :], in_=acc[m][hf][:, 0:258]), [acc[m][hf]], [aS])
                        else:
                            P.op('dve', lambda e: e.tensor_copy(out=aS[:, m * 2 + hf, :], in_=acc[m][hf][:, 0:258]), [acc[m][hf]], [aS])
                pg = post(c, qg, aS, mx)
        exhaust(pg)
        if cg is not None:
            for _ in cg:
                pass
        P.barrier()


def phase_l0_fourier(P, io, sc):
    with ExitStack() as es:
        cg = conv_gen(P, io, sc['uv'], es, layers=(1,), R=2, b0=0, b1=16)
        c4 = P.sb([128, 2, 128], BF16, 'c4', es=es)
        P.dma('sp', c4, c4[:], io['c4s4'], io['c4s4'][:, :, :])
        fT = P.sb([128, 2, NPOS], BF16, 'fT', es=es)
        for c in range(2):
            P.dma('sp', fT, fT[:, c, :], sc['fT'], sc['fT'][c, :, :])
        X = P.sb([128, 32, 512], BF16, 'Xcs', es=es)
        px = [P.ps([128, 512], F32, 'px', es=es) for _ in range(2)]
        for nc_ in range(32):
            p_ = px[nc_ % 2]
            for cs in range(2):
                for c in range(2):
                    P.op('pe', lambda e: e.matmul(p_[:, cs * 256 + c * 128: cs * 256 + (c + 1) * 128],
                                                  lhsT=fT[:, c, nc_ * 128:(nc_ + 1) * 128], rhs=c4[:, cs, :], start=True, stop=True), [fT, c4], [p_])
            eng = 'act' if nc_ % 2 == 0 else 'dve'
            if eng == 'act':
                P.op('act', lambda e: e.copy(out=X[:, nc_, :], in_=p_[:]), [p_], [X])
            else:
                P.op('dve', lambda e: e.tensor_copy(out=X[:, nc_, :], in_=p_[:]), [p_], [X])
        tab = [P.sb([128, 32, 512], BF16, 'tab', es=es) for _ in range(2)]
        py = [P.ps([128, 512], F32, 'py', es=es) for _ in range(2)]
        yo = [P.sb([128, 512], BF16, 'yo', es=es) for _ in range(2)]
        for kg in range(4):
            for cs in range(2):
                for n8 in range(4):
                    P.dma('sp', tab[cs], tab[cs][:, n8 * 8:(n8 + 1) * 8, :], io['dft'],
                          io['dft'][cs, n8 * 1024:(n8 + 1) * 1024, kg * 512:(kg + 1) * 512].rearrange("(nc p) k -> p nc k", p=128), nowaw=True)
            for c in range(2):
                p_ = py[c]
                i = 0
                for cs in range(2):
                    for nc_ in range(32):
                        P.op('pe', lambda e: e.matmul(p_[:], lhsT=X[:, nc_, cs * 256 + c * 128: cs * 256 + (c + 1) * 128],
                                                      rhs=tab[cs][:, nc_, :], start=(i == 0), stop=(i == 63)), [X, tab[cs]], [p_])
                        i += 1
                        if i % 8 == 0:
                            next(cg, None)
                o_ = yo[c]
                P.op('act', lambda e: e.activation(out=o_[:], in_=p_[:], func=AF.Identity, scale=1.0 / 512.0), [p_], [o_])
                P.store('sp', sc['mixT'][6 + c, :, kg * 512:(kg + 1) * 512], o_, o_[:])
        for _ in cg:
            pass
        P.barrier()


def phase_wout_resid(P, io, mods, w_b, mix_b, xin_b, xin_ap, xout_b, uv=None):
    with ExitStack() as es:
        wbf = load_weight_bf16(P, es, w_b, w_b[:, :], 1024, 'wout_bf')
        mt = [P.sb([128, 8, 128], BF16, 'mt', es=es) for _ in range(2)]
        xt = [P.sb([128, 1024], F32, 'xt', es=es) for _ in range(2)]
        yt = [P.sb([128, 1024], F32, 'yt', es=es) for _ in range(2)]
        py = [P.ps([128, 512], F32, 'py', es=es) for _ in range(4)]
        cg = conv_gen(P, io, uv, es, layers=(1,), R=2, b0=16, b1=24) if uv is not None else iter(())
        for t in range(NT):
            m_ = mt[t % 2]; x_ = xt[t % 2]; y_ = yt[t % 2]
            next(cg, None); next(cg, None)
            P.dma('sp', m_, m_[:], mix_b, mix_b[:, :, t * 128:(t + 1) * 128].rearrange("kc p t -> p kc t"))
            P.dma('sp', x_, x_[:], xin_b, xin_ap[t * 128:(t + 1) * 128, :])
            for hf in range(2):
                p_ = py[(2 * t + hf) % 4]
                for kc in range(8):
                    P.op('pe', lambda e: e.matmul(p_[:], lhsT=m_[:, kc, :], rhs=wbf[:, kc, hf * 512:(hf + 1) * 512],
                                                  start=(kc == 0), stop=(kc == 7)), [m_, wbf], [p_])
                sl = slice(hf * 512, (hf + 1) * 512)
                P.op('dve', lambda e: e.tensor_tensor(out=y_[:, sl], in0=p_[:], in1=mods['G1'][:, sl], op=ALU.mult), [p_, mods['G1']], [y_])
            P.op('pool', lambda e: e.tensor_tensor(out=y_[:], in0=y_[:], in1=x_[:], op=ALU.add), [y_, x_], [y_])
            P.store('sp', xout_b[t * 128:(t + 1) * 128, :], y_, y_[:])
        for _ in cg:
            pass
        P.barrier()


def conv_gen(P, io, uv, es, layers=(0, 1), R=4, b0=0, b1=None):
    su = [P.sb([128, R, 1024], F32, 'cvu', es=es) for _ in range(2)]
    sv = [P.sb([128, R, 1024], F32, 'cvv', es=es) for _ in range(2)]
    ob = [P.sb([128, R, 2048], BF16, 'cvo', es=es) for _ in range(2)]
    nblk = NEXP // (128 * R)
    i = 0
    for l in layers:
        for b in range(b0, nblk if b1 is None else b1):
            u_ = su[i % 2]; v_ = sv[i % 2]; o_ = ob[i % 2]; i += 1
            r0 = b * 128 * R
            P.dma('pool', u_, u_[:], io['peer_u'], io['peer_u'][l, r0:r0 + 128 * R, :].rearrange("(p r) d -> p r d", r=R))
            P.dma('pool', v_, v_[:], io['peer_v'], io['peer_v'][l, r0:r0 + 128 * R, :].rearrange("(p r) d -> p r d", r=R))
            yield
            P.op('pool', lambda e: e.tensor_copy(out=o_[:, :, 0:1024], in_=u_[:]), [u_], [o_])
            yield
            P.op('pool', lambda e: e.tensor_copy(out=o_[:, :, 1024:2048], in_=v_[:]), [v_], [o_])
            yield
            P.store('pool', uv[l * NEXP + r0: l * NEXP + r0 + 128 * R, :].rearrange("(p r) d -> p r d", r=R), o_, o_[:])
            yield


def phase_peer(P, io, l, mods, xin_b, xout_b, final, uv):
    with ExitStack() as es:
        identb = make_ident(P, es, BF16)
        wq = load_weight_bf16(P, es, io['peer_wq'], io['peer_wq'][l], 2048, 'wq_bf')
        kb = P.sb([128, 16, 128], BF16, 'kb', es=es)
        with ExitStack() as es2:
            kst = P.sb([128, 16, 128], F32, 'kst', es=es2)
            P.dma('sp', kst, kst[:], io['keysT'], io['keysT'][l])
            P.op('dve', lambda e: e.tensor_copy(out=kb[:], in_=kst[:]), [kst], [kb])
            P.barrier()
        cu = P.sb([128, 2], U32, 'cu', es=es)
        P.op('dve', lambda e: e.memset(cu[:, 0:1], 4), [], [cu])
        P.op('dve', lambda e: e.memset(cu[:, 1:2], 15), [], [cu])
        io16i = P.sb([128, 16], I32, 'io16i', es=es)
        P.op('pool', lambda e: e.iota(out=io16i[:], pattern=[[1, 16]], base=0, channel_multiplier=0), [], [io16i])
        io16 = P.sb([128, 16], F32, 'io16', es=es)
        P.op('dve', lambda e: e.tensor_copy(out=io16[:], in_=io16i[:]), [io16i], [io16])
        if final:
            fg = P.sb([128, 1024], F32, 'fg', es=es)
            P.dma('sp', fg, fg[:], io['final_g'], io['final_g'][0:1, :].partition_broadcast(128))
        xt = [P.sb([128, 1024], F32, 'xt', es=es) for _ in range(2)]
        tmp = P.sb([128, 1024], F32, 'tmp', es=es)
        tf = [P.sb([128, 1024], F32, 'tf', es=es) for _ in range(2)]
        tbs = [P.sb([128, 1024], BF16, 'tb', es=es) for _ in range(2)]
        prodb = [P.sb([128, 1024], BF16, 'prodb', es=es) for _ in range(4)]
        trailA = P.sb([128, 8], F32, 'trailA', es=es)
        ss = P.sb([128, 1], F32, 'ss', es=es); rstd = P.sb([128, 1], F32, 'rstd', es=es)
        ss2 = P.sb([128, 1], F32, 'ss2', es=es); rstd2 = P.sb([128, 1], F32, 'rstd2', es=es)
        pjunk = P.sb([128, 1024], BF16, 'pjunk', es=es)
        tT = P.sb([128, 8, 128], BF16, 'tT', es=es)
        qTb = P.sb([128, 16, 128], BF16, 'qTb', es=es)
        scs = P.sb([128, 16, 128], F32, 'scs', es=es)
        wk = P.sb([128, 2048], F32, 'wk', es=es)
        wk2 = P.sb([128, 2048], F32, 'wk2', es=es)
        tv = P.sb([128, 16, 16], F32, 'tv', es=es)
        ti = P.sb([128, 16, 16], U32, 'ti', es=es)
        tif = P.sb([128, 16, 16], F32, 'tif', es=es)
        cv = P.sb([128, 8, 16], F32, 'cv', es=es)
        ci = P.sb([128, 8, 16], U32, 'ci', es=es)
        au = P.sb([128, 8, 16], U32, 'au', es=es); bu = P.sb([128, 8, 16], U32, 'bu', es=es)
        af = P.sb([128, 8, 16], F32, 'af', es=es); bf = P.sb([128, 8, 16], F32, 'bf', es=es)
        sel = P.sb([128, 2, 8, 16], F32, 'sel', es=es)
        idxf = P.sb([128, 128], F32, 'idxf', es=es)
        idx = [P.sb([128, 128], U32, 'idx', es=es) for _ in range(2)]
        gt = [P.sb([128, 8, 16], F32, 'gt', es=es) for _ in range(2)]
        gs = P.sb([128, 8], F32, 'gs', es=es)
        trail = P.sb([128, 8], F32, 'trail', es=es)
        acc = [P.sb([128, 1024], F32, 'acc', es=es) for _ in range(2)]
        NB = 12 if final else 13
        gb = [P.sb([128, 2048], BF16, 'gb', es=es) for _ in range(NB)]
        dg = [P.sb([128, 128], BF16, 'dg', es=es) for _ in range(4)]
        dotsG = [P.sb([128, 4], F32, 'dotsG', es=es) for _ in range(3)]
        wgG = [P.sb([128, 4], F32, 'wgG', es=es) for _ in range(3)]
        ptr = P.ps([128, 8, 128], BF16, 'ptr', es=es)
        pq = [P.ps([128, 4, 128], F32, 'pq', es=es) for _ in range(2)]
        psc = [P.ps([128, 4, 128], F32, 'psc', es=es) for _ in range(2)]
        pacc = [P.ps([128, 512], F32, 'pacc', es=es) for _ in range(2)]

        scw_v = wk[:].rearrange("p (a b) -> p a b", b=128)
        candw_v = wk[:].rearrange("p (a b) -> p a b", b=256)
        cand_v = wk2[:].rearrange("p (a b) -> p a b", b=256)
        eq_v = wk2[:].rearrange("p (h a b) -> p h a b", a=16, b=16)

        def idle(n):
            for _ in range(n):
                yield

        def routing(t):
            x_ = xt[t % 2]; tf_ = tf[t % 2]; idx_ = idx[t % 2]; gt_ = gt[t % 2]; tb = tbs[t % 2]
            P.dma('sp', x_, x_[:], xin_b, xin_b[t * 128:(t + 1) * 128, :])
            yield from idle(10)
            P.op('dve', lambda e: e.scalar_tensor_tensor(out=tmp[:], in0=x_[:], scalar=1.0, in1=x_[:], op0=ALU.mult, op1=ALU.mult,
                                                         accum_out=ss[:]), [x_], [tmp, ss])
            P.op('dve', lambda e: e.memset(trail[:, 1:2], 0.0), [], [trail, ss]); yield
            yield
            P.op('act', lambda e: e.activation(out=ss[:], in_=ss[:], func=AF.Sqrt, scale=1.0 / 1024, bias=P.epsb[:]), [ss, P.epsb], [ss])
            yield from idle(3)
            P.op('dve', lambda e: e.reciprocal(out=rstd[:], in_=ss[:]), [ss], [rstd]); yield
            P.op('dve', lambda e: e.scalar_tensor_tensor(out=tmp[:], in0=x_[:], scalar=rstd[:, 0:1], in1=mods['A2'][:],
                                                         op0=ALU.mult, op1=ALU.mult), [x_, rstd, mods['A2']], [tmp]); yield
            P.op('dve', lambda e: e.tensor_tensor(out=tf_[:], in0=tmp[:], in1=mods['B2'][:], op=ALU.add), [tmp, mods['B2']], [tf_])
            yield from idle(2)
            P.op('act', lambda e: e.copy(out=tb[:], in_=tf_[:]), [tf_], [tb])
            yield from idle(2)
            transpose_tile(P, tb, ptr, identb, tT, tT[:])
            yield from idle(3)
            for g4 in range(4):
                p_ = pq[g4 % 2]
                for j in range(4):
                    hs = g4 * 4 + j
                    for kc in range(8):
                        P.op('pe', lambda e: e.matmul(p_[:, j, :], lhsT=wq[:, kc, hs * 128:(hs + 1) * 128], rhs=tT[:, kc, :],
                                                      start=(kc == 0), stop=(kc == 7)), [wq, tT], [p_])
                yield from idle(3)
                P.op('act', lambda e: e.copy(out=qTb[:, g4 * 4:(g4 + 1) * 4, :], in_=p_[:]), [p_], [qTb]); yield
            yield from idle(2)
            for g4 in range(4):
                p_ = psc[g4 % 2]
                for j in range(4):
                    hs = g4 * 4 + j
                    P.op('pe', lambda e: e.matmul(p_[:, j, :], lhsT=qTb[:, hs, :], rhs=kb[:, hs, :], start=True, stop=True), [qTb, kb], [p_])
                yield from idle(2)
                P.op('act', lambda e: e.copy(out=scs[:, g4 * 4:(g4 + 1) * 4, :], in_=p_[:]), [p_], [scs]); yield
            yield from idle(2)
            for hs in range(16):
                P.op('dve', lambda e: e.max(out=tv[:, hs, 0:8], in_=scs[:, hs, :]), [scs], [tv])
                P.op('dve', lambda e: e.max_index(out=ti[:, hs, 0:8], in_max=tv[:, hs, 0:8], in_values=scs[:, hs, :]), [scs, tv], [ti]); yield
                P.op('dve', lambda e: e.match_replace(out=scw_v[:, hs, :], in_to_replace=tv[:, hs, 0:8], in_values=scs[:, hs, :], imm_value=-1e30), [scs, tv], [wk])
                P.op('dve', lambda e: e.max(out=tv[:, hs, 8:16], in_=scw_v[:, hs, :]), [wk], [tv]); yield
                P.op('dve', lambda e: e.max_index(out=ti[:, hs, 8:16], in_max=tv[:, hs, 8:16], in_values=scw_v[:, hs, :]), [wk, tv], [ti]); yield
            P.op('dve', lambda e: e.tensor_copy(out=tif[:], in_=ti[:]), [ti], [tif])
            tvv = tv[:].rearrange("p (h s) k -> p h s k", s=2)
            tiv = tif[:].rearrange("p (h s) k -> p h s k", s=2)
            candv = wk2[:].rearrange("p (h a b) -> p h a b", a=16, b=16)
            P.op('dve', lambda e: e.tensor_tensor(out=candv, in0=tvv[:, :, 0, :].unsqueeze(3).to_broadcast([128, 8, 16, 16]),
                                                  in1=tvv[:, :, 1, :].unsqueeze(2).to_broadcast([128, 8, 16, 16]), op=ALU.add), [tv], [wk2]); yield
            for h in range(8):
                P.op('dve', lambda e: e.max(out=cv[:, h, 0:8], in_=cand_v[:, h, :]), [wk2], [cv])
                P.op('dve', lambda e: e.max_index(out=ci[:, h, 0:8], in_max=cv[:, h, 0:8], in_values=cand_v[:, h, :]), [wk2, cv], [ci]); yield
                P.op('dve', lambda e: e.match_replace(out=candw_v[:, h, :], in_to_replace=cv[:, h, 0:8], in_values=cand_v[:, h, :], imm_value=-1e30), [wk2, cv], [wk])
                P.op('dve', lambda e: e.max(out=cv[:, h, 8:16], in_=candw_v[:, h, :]), [wk], [cv]); yield
                P.op('dve', lambda e: e.max_index(out=ci[:, h, 8:16], in_max=cv[:, h, 8:16], in_values=candw_v[:, h, :]), [wk, cv], [ci]); yield
            P.op('dve', lambda e: e.tensor_scalar(out=au[:], in0=ci[:], scalar1=cu[:, 0:1], scalar2=None, op0=ALU.logical_shift_right), [ci, cu], [au])
            P.op('dve', lambda e: e.tensor_scalar(out=bu[:], in0=ci[:], scalar1=cu[:, 1:2], scalar2=None, op0=ALU.bitwise_and), [ci, cu], [bu]); yield
            P.op('dve', lambda e: e.tensor_copy(out=af[:], in_=au[:]), [au], [af])
            P.op('dve', lambda e: e.tensor_copy(out=bf[:], in_=bu[:]), [bu], [bf]); yield
            for s_, xf in ((0, af), (1, bf)):
                P.op('dve', lambda e: e.tensor_tensor(out=eq_v, in0=xf[:].unsqueeze(3).to_broadcast([128, 8, 16, 16]),
                                                      in1=io16[:].unsqueeze(1).unsqueeze(1).to_broadcast([128, 8, 16, 16]), op=ALU.is_equal), [xf, io16], [wk2]); yield
                P.op('dve', lambda e: e.tensor_tensor(out=eq_v, in0=eq_v, in1=tiv[:, :, s_, :].unsqueeze(2).to_broadcast([128, 8, 16, 16]), op=ALU.mult), [wk2, tif], [wk2]); yield
                P.op('dve', lambda e: e.tensor_reduce(out=sel[:, s_, :, :], in_=eq_v, axis=AX.X, op=ALU.add), [wk2], [sel]); yield
            P.op('dve', lambda e: e.scalar_tensor_tensor(out=idxf[:], in0=sel[:, 0, :, :].rearrange("p h k -> p (h k)"), scalar=128.0,
                                                         in1=sel[:, 1, :, :].rearrange("p h k -> p (h k)"), op0=ALU.mult, op1=ALU.add), [sel], [idxf])
            if l > 0:
                P.op('dve', lambda e: e.tensor_scalar(out=idxf[:], in0=idxf[:], scalar1=float(l * NEXP), scalar2=None, op0=ALU.add), [idxf], [idxf])
            P.op('dve', lambda e: e.tensor_copy(out=idx_[:], in_=idxf[:]), [idxf], [idx_]); yield
            P.op('dve', lambda e: e.tensor_tensor(out=gt_[:], in0=cv[:], in1=cv[:, :, 0:1].to_broadcast([128, 8, 16]), op=ALU.subtract), [cv], [gt_])
            P.op('act', lambda e: e.activation(out=gt_[:], in_=gt_[:], func=AF.Exp), [gt_], [gt_]); yield
            P.op('dve', lambda e: e.tensor_reduce(out=gs[:], in_=gt_[:], axis=AX.X, op=ALU.add), [gt_], [gs])
            P.op('dve', lambda e: e.reciprocal(out=gs[:], in_=gs[:]), [gs], [gs]); yield
            P.op('dve', lambda e: e.tensor_tensor(out=gt_[:], in0=gt_[:], in1=gs[:].unsqueeze(2).to_broadcast([128, 8, 16]), op=ALU.mult), [gt_, gs], [gt_]); yield

        def step(gen):
            if gen is not None:
                next(gen, None)

        def exhaust(gen):
            if gen is not None:
                for _ in gen:
                    pass

        exhaust(routing(0))
        ig = 0; iv = 0; ip = 0
        P.op('act', lambda e: e.copy(out=trailA[:], in_=trail[:]), [trail], [trailA])
        for t in range(NT):
            x_ = xt[t % 2]; tf_ = tf[t % 2]; idx_ = idx[t % 2]; gt_ = gt[t % 2]; acc_ = acc[t % 2]; tb_ = tbs[t % 2]
            nxt = routing(t + 1) if t + 1 < NT else None
            slots = {}
            for gi in range(33):
                if gi < 32:
                    dg_ = dotsG[gi % 3]
                    for s4 in range(4):
                        j = gi * 4 + s4
                        g_ = gb[ig % NB]; ig += 1
                        slots[j] = g_
                        P.dma('pool', g_, None, uv, None,
                              fn=lambda e: e.indirect_dma_start(out=g_[:], out_offset=None, in_=uv[:, :],
                                                                in_offset=bass.IndirectOffsetOnAxis(ap=idx_[:, j:j + 1], axis=0)), extra_reads=[idx_])
                        pr_ = prodb[ip % 4]; ip += 1
                        P.op('dve', lambda e: e.tensor_tensor(out=pr_[:], in0=g_[:, 0:1024], in1=tb_[:], op=ALU.mult), [g_, tb_], [pr_])
                        P.op('act', lambda e: e.activation(out=pjunk[:], in_=pr_[:], func=AF.Identity, accum_out=dg_[:, s4:s4 + 1]),
                             [pr_], ([dg_] if s4 == 0 else []))
                        step(nxt)
                        step(nxt)
                    P.op('act', lambda e: e.copy(out=trailA[:, 0:1], in_=trailA[:, 1:2]), [], [trailA, dg_])
                if gi >= 1:
                    gp = gi - 1
                    dp_ = dotsG[gp % 3]; w_ = wgG[gp % 3]
                    P.op('act', lambda e: e.activation(out=w_[:], in_=dp_[:], func=AF.Gelu_apprx_tanh), [dp_], [w_])
                    P.op('dve', lambda e: e.tensor_tensor(out=w_[:], in0=w_[:], in1=gt_[:].rearrange("p h k -> p (h k)")[:, gp * 4:gp * 4 + 4], op=ALU.mult), [w_, gt_], [w_])
                    for s4 in range(4):
                        j = gp * 4 + s4
                        g_ = slots.pop(j)
                        d_ = dg[iv % 4]; iv += 1
                        P.op('dve', lambda e: e.tensor_scalar(out=d_[:], in0=identb[:], scalar1=w_[:, s4:s4 + 1], scalar2=None, op0=ALU.mult), [identb, w_], [d_])
                        for hf in range(2):
                            P.op('pe', lambda e: e.matmul(pacc[hf][:], lhsT=d_[:], rhs=g_[:, 1024 + hf * 512:1024 + (hf + 1) * 512],
                                                          start=(j == 0), stop=(j == 127)), [d_, g_], [pacc[hf]])
            exhaust(nxt)
            for hf in range(2):
                sl = slice(hf * 512, (hf + 1) * 512)
                P.op('dve', lambda e: e.tensor_tensor(out=acc_[:, sl], in0=pacc[hf][:], in1=mods['G2'][:, sl], op=ALU.mult), [pacc[hf], mods['G2']], [acc_])
            P.op('dve', lambda e: e.tensor_tensor(out=acc_[:], in0=acc_[:], in1=x_[:], op=ALU.add), [acc_, x_], [acc_])
            if final:
                rms_rstd(P, acc_[:], 1024, ss2, rstd2, tmp, [acc_])
                P.op('dve', lambda e: e.scalar_tensor_tensor(out=acc_[:], in0=acc_[:], scalar=rstd2[:, 0:1], in1=fg[:],
                                                             op0=ALU.mult, op1=ALU.mult), [acc_, rstd2, fg], [acc_])
            P.store('sp', xout_b[t * 128:(t + 1) * 128, :], acc_, acc_[:])
        P.barrier()


def phase_l1_sgu(P, io, mods, xin_b, xout_b, uv=None):
    with ExitStack() as es:
        identb = make_ident(P, es, BF16)
        win = load_weight_bf16(P, es, io['odd_w_in'], io['odd_w_in'][:, :], 2048, 'oddwin_bf')
        wout = load_weight_bf16(P, es, io['odd_w_out'], io['odd_w_out'][:, :], 1024, 'oddwout_bf')
        wst = P.sb([128, 8, 128], F32, 'wst', es=es)
        P.dma('sp', wst, wst[:], io['wsT'], io['wsT'][:, :, :])
        wsb = P.sb([128, 8, 128], BF16, 'wsb', es=es)
        P.op('dve', lambda e: e.tensor_copy(out=wsb[:], in_=wst[:]), [wst], [wsb])
        bsT = P.sb([128, 8], F32, 'bsT', es=es)
        P.dma('sp', bsT, bsT[:], io['bsT'], io['bsT'][:, :])
        binb = P.sb([128, 2048], F32, 'binb', es=es)
        P.dma('sp', binb, binb[:], io['odd_b_in'], io['odd_b_in'][0:1, :].partition_broadcast(128))
        ngb = P.sb([128, 1024], F32, 'ngb', es=es)
        P.dma('sp', ngb, ngb[:], io['sgu_norm_g'], io['sgu_norm_g'][0:1, :].partition_broadcast(128))
        xt = [P.sb([128, 1024], F32, 'xt', es=es) for _ in range(2)]
        tmp = P.sb([128, 1024], F32, 'tmp', es=es)
        hbf = P.sb([128, 1024], BF16, 'hbf', es=es)
        ss = P.sb([128, 1], F32, 'ss', es=es); rstd = P.sb([128, 1], F32, 'rstd', es=es); junk = P.sb([128, 1024], F32, 'junk', es=es)
        hT = P.sb([128, 8, 128], BF16, 'hT', es=es)
        zf = P.sb([128, 2048], F32, 'zf', es=es)
        vn = P.sb([128, 1024], BF16, 'vn', es=es)
        gat = P.sb([128, 1024], BF16, 'gat', es=es)
        gT = P.sb([128, 8, 128], BF16, 'gT', es=es)
        yt = [P.sb([128, 1024], F32, 'yt', es=es) for _ in range(2)]
        ptr = P.ps([128, 8, 128], BF16, 'ptr', es=es)
        pz = [P.ps([128, 512], F32, 'pz', es=es) for _ in range(4)]
        psg = [P.ps([128, 4, 128], F32, 'psg', es=es) for _ in range(2)]
        cg = conv_gen(P, io, uv, es, layers=(1,), R=2, b0=24) if uv is not None else None

        def stepn(n):
            if cg is not None:
                for _ in range(n):
                    next(cg, None)
        for t in range(NT):
            x_ = xt[t % 2]; y_ = yt[t % 2]
            stepn(3)
            P.dma('sp', x_, x_[:], xin_b, xin_b[t * 128:(t + 1) * 128, :])
            norm_mod_tile(P, x_, mods['A1'], mods['B1'], (ss, rstd, junk), tmp, hbf)
            transpose_tile(P, hbf, ptr, identb, hT, hT[:])
            for n4 in range(4):
                p_ = pz[n4]
                for kc in range(8):
                    P.op('pe', lambda e: e.matmul(p_[:], lhsT=hT[:, kc, :], rhs=win[:, kc, n4 * 512:(n4 + 1) * 512],
                                                  start=(kc == 0), stop=(kc == 7)), [hT, win], [p_])
                sl = slice(n4 * 512, (n4 + 1) * 512)
                P.op('dve', lambda e: e.tensor_tensor(out=zf[:, sl], in0=p_[:], in1=binb[:, sl], op=ALU.add), [p_, binb], [zf])
            stepn(3)
            P.op('act', lambda e: e.activation(out=zf[:], in_=zf[:], func=AF.Gelu_apprx_tanh), [zf], [zf])
            rms_rstd(P, zf[:, 1024:2048], 1024, ss, rstd, junk, [zf])
            P.op('dve', lambda e: e.scalar_tensor_tensor(out=vn[:], in0=zf[:, 1024:2048], scalar=rstd[:, 0:1], in1=ngb[:],
                                                         op0=ALU.mult, op1=ALU.mult), [zf, rstd, ngb], [vn])
            for g in range(8):
                p_ = psg[g // 4]
                P.op('pe', lambda e: e.matmul(p_[:, g % 4, :], lhsT=wsb[:, g, :], rhs=vn[:, g * 128:(g + 1) * 128], start=True, stop=True), [wsb, vn], [p_])
            for g in range(8):
                p_ = psg[g // 4]
                P.op('dve', lambda e: e.scalar_tensor_tensor(out=gat[:, g * 128:(g + 1) * 128], in0=p_[:, g % 4, :], scalar=bsT[:, g:g + 1],
                                                             in1=zf[:, g * 128:(g + 1) * 128], op0=ALU.add, op1=ALU.mult), [p_, bsT, zf], [gat])
            if t == 0 and getattr(P, 'dbg_sgu', None) is not None:
                dd_ = P.dbg_sgu
                P.dma('sp', dd_, dd_[0, :, :], zf, zf[:], nowaw=True)
                vf_ = P.sb([128, 2048], F32, 'dbgvf', es=es)
                P.op('dve', lambda e: e.tensor_copy(out=vf_[:, 0:1024], in_=vn[:]), [vn], [vf_])
                P.op('dve', lambda e: e.tensor_copy(out=vf_[:, 1024:2048], in_=gat[:]), [gat], [vf_])
                P.dma('sp', dd_, dd_[1, :, :], vf_, vf_[:], nowaw=True)
            stepn(2)
            transpose_tile(P, gat, ptr, identb, gT, gT[:])
            stepn(2)
            for hf in range(2):
                p_ = pz[hf]
                for kc in range(8):
                    P.op('pe', lambda e: e.matmul(p_[:], lhsT=gT[:, kc, :], rhs=wout[:, kc, hf * 512:(hf + 1) * 512],
                                                  start=(kc == 0), stop=(kc == 7)), [gT, wout], [p_])
                sl = slice(hf * 512, (hf + 1) * 512)
                P.op('dve', lambda e: e.tensor_tensor(out=y_[:, sl], in0=p_[:], in1=mods['G1'][:, sl], op=ALU.mult), [p_, mods['G1']], [y_])
            P.op('pool', lambda e: e.tensor_tensor(out=y_[:], in0=y_[:], in1=x_[:], op=ALU.add), [y_, x_], [y_])
            P.store('sp', xout_b[t * 128:(t + 1) * 128, :], y_, y_[:])
        if cg is not None:
            for _ in cg:
                pass
        P.barrier()


IN_SPECS = [
    ('x', [NPOS, D], F32), ('ctx', [NCTX, D], F32), ('cT', [128, 8], F32), ('ccT', [128, 8], F32),
    ('ada_w', [2, D, 6 * D], F32), ('ada_b', [2, 6 * D], F32), ('norm1_g', [2, D], F32), ('norm2_g', [2, D], F32),
    ('final_g', [1, D], F32), ('w_in', [D, 4096], F32), ('w_out', [D, D], F32), ('dlam', [1, 256], F32), ('head_g', [1, 128], F32),
    ('cosT', [128, NPOS], F32), ('sinT', [128, NPOS], F32), ('c4s4', [128, 2, 128], BF16), ('dft', [2, NPOS, OWN], BF16),
    ('odd_w_in', [D, 2048], F32), ('odd_b_in', [1, 2048], F32), ('sgu_norm_g', [1, D], F32), ('wsT', [128, 8, 128], F32),
    ('bsT', [128, 8], F32), ('odd_w_out', [D, D], F32),
    ('peer_wq', [2, D, 2048], F32), ('keysT', [2, 128, 16, 128], F32), ('peer_u', [2, NEXP, D], F32), ('peer_v', [2, NEXP, D], F32),
]
TEST_SMALL_PEER = False


def build(stop=None, debug_outs=()):
    nc = bass.Bass("TRN2", target_bir_lowering=False)
    with ExitStack() as es:
        P = Prog(nc, es)
        io = {}
        for name, shape, dt in IN_SPECS:
            if TEST_SMALL_PEER and name in ('peer_u', 'peer_v'):
                shape = [2, 128, D]
            io[name] = P.dram(name, shape, dt, kind="ExternalInput")
        out = P.dram('out', [OWN, D], F32, kind="ExternalOutput")
        def scr(name, shape, dt):
            return P.dram(name, shape, dt, kind=("ExternalOutput" if name in debug_outs else "Internal"))
        sc = {
            'kT': scr('kT_d', [6, 128, NKEY], BF16), 'v': scr('v_d', [NKEY, 768], BF16), 'qT': scr('qT_d', [6, 128, OWN], BF16),
            'fT': scr('fT_d', [2, 128, NPOS], BF16), 'mixT': scr('mixT_d', [8, 128, OWN], BF16),
            'uv': scr('uv_d', [2 * NEXP, 2 * D], BF16),
            'x1': scr('x1_d', [OWN, D], F32), 'x2': scr('x2_d', [OWN, D], F32), 'x3': scr('x3_d', [OWN, D], F32),
        }
        P.epsb = P.sb([128, 1], F32, 'epsb')
        P.op('dve', lambda e: e.memset(P.epsb[:], EPS), [], [P.epsb])
        mods = {n: P.sb([128, 1024], F32, 'mod_' + n) for n in ['A1', 'B1', 'G1', 'A2', 'B2', 'G2', 'A1c', 'B1c']}
        finals = [out]

        def run():
            phase_mod(P, io, 0, mods, True)
            if 'mods_d' in debug_outs:
                md = P.dram('mods_d', [8, 128, 1024], F32, kind="ExternalOutput")
                for i, n in enumerate(['A1', 'B1', 'G1', 'A2', 'B2', 'G2', 'A1c', 'B1c']):
                    P.dma('sp', md, md[i, :, :], mods[n], mods[n][:], nowaw=True)
                finals.append(md)
            if stop == 'mod0': return
            phase_l0_proj(P, io, mods, sc)
            if stop == 'proj': return
            phase_l0_attn(P, io, sc)
            if stop == 'attn': return
            phase_l0_fourier(P, io, sc)
            if stop == 'fourier': return
            phase_wout_resid(P, io, mods, io['w_out'], sc['mixT'], io['x'], io['x'][0:OWN, :], sc['x1'], sc['uv'])
            if stop == 'x1': return
            phase_peer(P, io, 0, mods, sc['x1'], sc['x2'], False, sc['uv'])
            if stop == 'x2': return
            phase_mod(P, io, 1, mods, False)
            if 'sgu_d' in debug_outs:
                P.dbg_sgu = P.dram('sgu_d', [2, 128, 2048], F32, kind="ExternalOutput"); finals.append(P.dbg_sgu)
            phase_l1_sgu(P, io, mods, sc['x2'], sc['x3'], sc['uv'])
            if stop == 'x3': return
            phase_peer(P, io, 1, mods, sc['x3'], out, True, sc['uv'])
        run()
        P.finish(list(sc.values()) + finals)
    return nc


def host_consts():
    pos = np.arange(NPOS)
    r = (pos // 64).astype(np.float32); col = (pos % 64).astype(np.float32)
    inv = (np.float32(10000.0) ** (-np.arange(16, dtype=np.float32) / np.float32(16))).astype(np.float32)
    ar = r[:, None] * inv; ac = col[:, None] * inv
    ang = np.concatenate([ar, ar, ac, ac], axis=-1).astype(np.float32)
    cos = np.cos(ang).astype(np.float32); sin = np.sin(ang).astype(np.float32)
    sign = np.concatenate([-np.ones(16), np.ones(16), -np.ones(16), np.ones(16)]).astype(np.float32)
    cosT = np.ascontiguousarray(np.concatenate([cos.T, cos.T], axis=0))
    sinT = np.ascontiguousarray(np.concatenate([(sin * sign).T, (sin * sign).T], axis=0))
    src = np.concatenate([np.arange(16, 32), np.arange(0, 16), np.arange(48, 64), np.arange(32, 48)])
    jj = np.arange(64)
    a4 = 2.0 * np.pi * np.outer(jj, jj) / 64.0
    c4 = np.zeros((128, 2, 128), np.float64)
    for b in range(2):
        c4[b * 64:(b + 1) * 64, 0, b * 64:(b + 1) * 64] = np.cos(a4)
        c4[b * 64:(b + 1) * 64, 1, b * 64:(b + 1) * 64] = -np.sin(a4)
    c4 = c4.astype(ml_dtypes.bfloat16)
    dfts = []
    for half in range(2):
        n = (np.arange(NPOS, dtype=np.int64) + half * OWN) % NPOS
        k = np.arange(half * OWN, (half + 1) * OWN, dtype=np.int64)
        ph = (np.outer(n, k) % NPOS).astype(np.float64) * (2.0 * np.pi / NPOS)
        dfts.append(np.stack([np.cos(ph), np.sin(ph)]).astype(ml_dtypes.bfloat16))
    return cosT, sinT, src, c4, dfts


def make_in_maps(inp, cores):
    cosT, sinT, src, c4, dfts = host_consts()
    w_in = inp['even_w_in'][0]
    permcols = np.concatenate([h * 64 + src for h in range(12)])
    w_ext = np.ascontiguousarray(np.concatenate([w_in, w_in[:, 0:768][:, permcols], w_in[:, 768:1536][:, permcols]], axis=1))
    keysT = np.ascontiguousarray(inp['peer_keys'].reshape(2, 16, 128, 128).transpose(0, 3, 1, 2))
    shared = {
        'ccT': np.ascontiguousarray(inp['c_ctx'].reshape(8, 128).T),
        'ada_w': inp['ada_w'], 'ada_b': inp['ada_b'], 'norm1_g': inp['norm1_g'], 'norm2_g': inp['norm2_g'],
        'final_g': inp['final_g'].reshape(1, D), 'w_in': w_ext, 'w_out': inp['even_w_out'][0],
        'dlam': inp['diff_lambda'].reshape(1, 256), 'head_g': inp['diff_norm_g'].reshape(1, 128),
        'cosT': cosT, 'sinT': sinT, 'c4s4': c4,
        'odd_w_in': inp['odd_w_in'][0], 'odd_b_in': inp['odd_b_in'].reshape(1, 2048), 'sgu_norm_g': inp['sgu_norm_g'].reshape(1, D),
        'wsT': np.ascontiguousarray(inp['sgu_w'][0].transpose(2, 0, 1)), 'bsT': np.ascontiguousarray(inp['sgu_b'][0].T),
        'odd_w_out': inp['odd_w_out'][0], 'peer_wq': inp['peer_wq'], 'keysT': keysT, 'peer_u': inp['peer_u'], 'peer_v': inp['peer_v'],
    }
    maps = []
    for core in cores:
        b, half = core // 2, core % 2
        m = dict(shared)
        order = (np.arange(NPOS) + half * OWN) % NPOS
        m['x'] = np.ascontiguousarray(inp['x'][b][order]); m['cosT'] = np.ascontiguousarray(cosT[:, order])
        m['sinT'] = np.ascontiguousarray(sinT[:, order]); m['ctx'] = np.ascontiguousarray(inp['ctx'][b])
        m['cT'] = np.ascontiguousarray(inp['c'][b].reshape(8, 128).T)
        m['dft'] = dfts[half]
        maps.append(m)
    return maps


def kernel(**inputs):
    inp = {k: np.asarray(v) for k, v in inputs.items()}
    nc = build()
    maps = make_in_maps(inp, list(range(8)))
    res = run_bass_kernel_spmd(nc, maps, core_ids=list(range(8)))
    out = np.zeros((4, NPOS, D), np.float32)
    for core in range(8):
        b, half = core // 2, core % 2
        out[b, half * OWN:(half + 1) * OWN, :] = res.results[core]['out']
    return out
```
